# Optimizing a Trainium2 kernel written in Bass

```python
import jax, jax.numpy as jnp
from jax import lax
import numpy as np

D_MODEL = 1024
BATCH = 32
SEQ = 2048
DEPTH = 4

CHUNK = 64
RMS_EPS = 1e-6
M_HEADS = 4
M_HEAD_DIM = D_MODEL // 4
M_WIDTH = M_HEADS * M_HEAD_DIM
CONV_K = 4
P_GROUPS = 4
P_WINDOWS = (2, 4, 8, 16)
P_GROUP_DIM = D_MODEL // 4
P_WIDTH = P_GROUPS * P_GROUP_DIM
A_IN = 3 * M_WIDTH + 2 * M_HEADS + 2 * P_WIDTH
A_OUT_IN = M_WIDTH + P_WIDTH
C_HEADS = 8
QK_NOPE = 128
QK_ROPE = 64
QK_HEAD = QK_NOPE + QK_ROPE
V_HEAD = 128
Q_LORA = 384
KV_LORA = 256
C_WIDTH = C_HEADS * V_HEAD
C_IN = Q_LORA + KV_LORA + QK_ROPE + C_WIDTH
ROPE_THETA = 10000.0
Q_BLOCK = 128
N_A = (DEPTH + 1) // 2
N_C = DEPTH // 2

kernel_name = 'hybrid_mlstm_pool_mla_stream'


def rms_norm(x, g):
    xf = x.astype(jnp.float32)
    y = xf * lax.rsqrt(jnp.mean(xf * xf, axis=-1, keepdims=True) + RMS_EPS)
    return (y * g.astype(jnp.float32)).astype(x.dtype)


def causal_conv(x, w, b):
    y = lax.conv_general_dilated(x, w[:, None, :].astype(x.dtype), window_strides=(1,),
                                 padding=[(CONV_K - 1, 0)],
                                 dimension_numbers=('NWC', 'WIO', 'NWC'),
                                 feature_group_count=x.shape[-1])
    return y + b


def mlstm_chunkwise(q, k, v, i_pre, log_f):
    B, H, S, Dk = q.shape
    Dv = v.shape[-1]
    nc = S // CHUNK

    def to_chunks(a):
        return jnp.moveaxis(a.reshape(B, H, nc, CHUNK, *a.shape[3:]), 2, 0)

    xs = (to_chunks(q), to_chunks(k), to_chunks(v), to_chunks(i_pre), to_chunks(log_f))
    tri = jnp.tril(jnp.ones((CHUNK, CHUNK), dtype=bool))

    def step(carry, inp):
        C, n, m = carry
        qb, kb, vb, ib, fb = inp
        bcum = jnp.cumsum(fb, axis=-1)
        dmat = jnp.where(tri, bcum[..., :, None] - bcum[..., None, :] + ib[..., None, :], -jnp.inf)
        inter = bcum + m[..., None]
        m_t = jnp.maximum(inter, jnp.max(dmat, axis=-1))
        w_inter = jnp.exp(inter - m_t)
        s = jnp.einsum('bhtd,bhsd->bhts', qb, kb) * jnp.exp(dmat - m_t[..., None])
        num = jnp.einsum('bhts,bhsv->bhtv', s, vb) + w_inter[..., None] * jnp.einsum('bhtd,bhdv->bhtv', qb, C)
        den = jnp.sum(s, axis=-1) + w_inter * jnp.einsum('bhtd,bhd->bht', qb, n)
        h = num / jnp.maximum(jnp.abs(den), jnp.exp(-m_t))[..., None]
        b_last = bcum[..., -1]
        g = b_last[..., None] - bcum + ib
        m_new = jnp.maximum(b_last + m, jnp.max(g, axis=-1))
        ws = jnp.exp(g - m_new[..., None])
        wc = jnp.exp(b_last + m - m_new)
        kw = kb * ws[..., None]
        C_new = wc[..., None, None] * C + jnp.einsum('bhsd,bhsv->bhdv', kw, vb)
        n_new = wc[..., None] * n + jnp.sum(kw, axis=2)
        return (C_new, n_new, m_new), h

    init = (jnp.zeros((B, H, Dk, Dv), jnp.float32), jnp.zeros((B, H, Dk), jnp.float32),
            jnp.zeros((B, H), jnp.float32))
    _, hs = lax.scan(step, init, xs)
    return jnp.moveaxis(hs, 0, 2).reshape(B, H, S, Dv)


def multiscale_pool(xp):
    B, S, _ = xp.shape
    xf = xp.astype(jnp.float32)
    cs = jnp.pad(jnp.cumsum(xf, axis=1), ((0, 0), (1, 0), (0, 0)))
    t = jnp.arange(S)
    means = []
    for g, w in enumerate(P_WINDOWS):
        csg = cs[..., g * P_GROUP_DIM:(g + 1) * P_GROUP_DIM]
        lo = jnp.maximum(t + 1 - w, 0)
        cnt = jnp.minimum(t + 1, w).astype(jnp.float32)
        means.append((csg[:, 1:] - jnp.take(csg, lo, axis=1)) / cnt[None, :, None])
    return (jnp.concatenate(means, axis=-1) - xf).astype(xp.dtype)


def mlstm_pool_layer(x, norm_g, w_in, b_if, conv_w, conv_b, w_q, w_k, w_v, head_norm_g, skip,
                     pool_w, pool_scale, w_out):
    B, S, _ = x.shape
    u = rms_norm(x, norm_g) @ w_in
    s1 = M_WIDTH
    s2 = 2 * M_WIDTH
    s3 = s2 + 2 * M_HEADS
    s4 = s3 + M_WIDTH
    s5 = s4 + P_WIDTH
    xm, om, gates, zm, xp, zp = jnp.split(u, [s1, s2, s3, s4, s5], axis=-1)
    xc = jax.nn.silu(causal_conv(xm, conv_w, conv_b))
    xc_h = xc.reshape(B, S, M_HEADS, M_HEAD_DIM)
    xm_h = xm.reshape(B, S, M_HEADS, M_HEAD_DIM)
    q = jnp.einsum('bshc,hcd->bhsd', xc_h, w_q).astype(jnp.float32)
    k = jnp.einsum('bshc,hcd->bhsd', xc_h, w_k).astype(jnp.float32) * (M_HEAD_DIM ** -0.5)
    v = jnp.einsum('bshc,hcd->bhsd', xm_h, w_v).astype(jnp.float32)
    gates = (gates + b_if).astype(jnp.float32)
    i_pre = jnp.moveaxis(gates[..., :M_HEADS], -1, 1)
    log_f = jnp.moveaxis(jax.nn.log_sigmoid(gates[..., M_HEADS:]), -1, 1)
    hm = mlstm_chunkwise(q, k, v, i_pre, log_f)
    hm = jnp.moveaxis(hm, 1, 2).reshape(B, S, M_WIDTH).astype(x.dtype) * jax.nn.sigmoid(om)
    hm = rms_norm(hm.reshape(B, S, M_HEADS, M_HEAD_DIM),
                  head_norm_g.reshape(M_HEADS, M_HEAD_DIM)).reshape(B, S, M_WIDTH)
    ya = (hm + skip * xc) * jax.nn.silu(zm)
    mix = multiscale_pool(xp).reshape(B, S, P_GROUPS, P_GROUP_DIM)
    yb = jnp.einsum('bsgc,gcd->bsgd', mix, pool_w).reshape(B, S, P_WIDTH) * pool_scale * jax.nn.silu(zp)
    return x + jnp.concatenate([ya, yb], axis=-1) @ w_out


def rope(x, cos, sin):
    x1, x2 = jnp.split(x.astype(jnp.float32), 2, axis=-1)
    return jnp.concatenate([x1 * cos - x2 * sin, x2 * cos + x1 * sin], axis=-1).astype(x.dtype)


def block_causal_attention(q, k, v):
    B, S, H, _ = q.shape
    scale = QK_HEAD ** -0.5
    chunk_id = jnp.arange(S) // CHUNK
    outs = []
    for qb in range(S // Q_BLOCK):
        q0, q1 = qb * Q_BLOCK, (qb + 1) * Q_BLOCK
        s = jnp.einsum('bqhd,bkhd->bhqk', q[:, q0:q1], k[:, :q1],
                       preferred_element_type=jnp.float32) * scale
        mask = chunk_id[q0:q1, None] >= chunk_id[None, :q1]
        p = jax.nn.softmax(jnp.where(mask, s, -jnp.inf), axis=-1).astype(v.dtype)
        outs.append(jnp.einsum('bhqk,bkhd->bqhd', p, v[:, :q1]))
    return jnp.concatenate(outs, axis=1)


def mla_layer(x, positions, norm_g, w_in, q_norm_g, kv_norm_g, w_uq, w_ukv, qn_g, kn_g, w_out):
    B, S, _ = x.shape
    u = rms_norm(x, norm_g) @ w_in
    cq, ckv, kpe, z = jnp.split(u, [Q_LORA, Q_LORA + KV_LORA, Q_LORA + KV_LORA + QK_ROPE], axis=-1)
    q = (rms_norm(cq, q_norm_g) @ w_uq).reshape(B, S, C_HEADS, QK_HEAD)
    kv = (rms_norm(ckv, kv_norm_g) @ w_ukv).reshape(B, S, C_HEADS, QK_NOPE + V_HEAD)
    k_nope, v = jnp.split(kv, [QK_NOPE], axis=-1)
    k = jnp.concatenate([k_nope, jnp.broadcast_to(kpe[:, :, None, :], (B, S, C_HEADS, QK_ROPE))], axis=-1)
    q = rms_norm(q, qn_g)
    k = rms_norm(k, kn_g)
    inv_freq = ROPE_THETA ** (-jnp.arange(0, QK_ROPE, 2, dtype=jnp.float32) / QK_ROPE)
    ang = positions.astype(jnp.float32)[..., None] * inv_freq
    cos = jnp.cos(ang)[:, :, None, :]
    sin = jnp.sin(ang)[:, :, None, :]
    q = jnp.concatenate([q[..., :QK_NOPE], rope(q[..., QK_NOPE:], cos, sin)], axis=-1)
    k = jnp.concatenate([k[..., :QK_NOPE], rope(k[..., QK_NOPE:], cos, sin)], axis=-1)
    o = block_causal_attention(q, k, v).reshape(B, S, C_WIDTH) * jax.nn.silu(z)
    return x + o @ w_out


def setup_inputs(seed: int = 0) -> dict:
    key = jax.random.key(seed)
    ks = list(jax.random.split(key, 32))
    f32 = jnp.float32

    def normal(shape, scale):
        return jax.random.normal(ks.pop(), shape, f32) * scale

    def gain(shape):
        return 1.0 + normal(shape, 0.02)

    x = normal((BATCH, SEQ, D_MODEL), 1.0)
    offsets = jax.random.randint(ks.pop(), (BATCH, 1), 0, 64) * CHUNK
    positions = (offsets + jnp.arange(SEQ)[None, :]).astype(jnp.int32)
    a_b_if = jnp.concatenate([normal((N_A, M_HEADS), 0.1),
                              jnp.linspace(3.0, 6.0, M_HEADS)[None, :] + normal((N_A, M_HEADS), 0.1)], axis=-1)
    return {
        'x': x,
        'positions': positions,
        'a_norm_g': gain((N_A, D_MODEL)),
        'a_w_in': normal((N_A, D_MODEL, A_IN), D_MODEL ** -0.5),
        'a_b_if': a_b_if,
        'a_conv_w': normal((N_A, CONV_K, M_WIDTH), CONV_K ** -0.5),
        'a_conv_b': normal((N_A, M_WIDTH), 0.02),
        'a_w_q': normal((N_A, M_HEADS, M_HEAD_DIM, M_HEAD_DIM), M_HEAD_DIM ** -0.5),
        'a_w_k': normal((N_A, M_HEADS, M_HEAD_DIM, M_HEAD_DIM), M_HEAD_DIM ** -0.5),
        'a_w_v': normal((N_A, M_HEADS, M_HEAD_DIM, M_HEAD_DIM), M_HEAD_DIM ** -0.5),
        'a_head_norm_g': gain((N_A, M_WIDTH)),
        'a_skip': gain((N_A, M_WIDTH)),
        'a_pool_w': normal((N_A, P_GROUPS, P_GROUP_DIM, P_GROUP_DIM), P_GROUP_DIM ** -0.5),
        'a_pool_scale': 1.0 + normal((N_A, P_WIDTH), 0.1),
        'a_w_out': normal((N_A, A_OUT_IN, D_MODEL), A_OUT_IN ** -0.5),
        'c_norm_g': gain((N_C, D_MODEL)),
        'c_w_in': normal((N_C, D_MODEL, C_IN), D_MODEL ** -0.5),
        'c_q_norm_g': gain((N_C, Q_LORA)),
        'c_kv_norm_g': gain((N_C, KV_LORA)),
        'c_w_uq': normal((N_C, Q_LORA, C_HEADS * QK_HEAD), Q_LORA ** -0.5),
        'c_w_ukv': normal((N_C, KV_LORA, C_HEADS * (QK_NOPE + V_HEAD)), KV_LORA ** -0.5),
        'c_qn_g': gain((N_C, QK_HEAD)),
        'c_kn_g': gain((N_C, QK_HEAD)),
        'c_w_out': normal((N_C, C_WIDTH, D_MODEL), C_WIDTH ** -0.5),
    }


def reference(x, positions, a_norm_g, a_w_in, a_b_if, a_conv_w, a_conv_b, a_w_q, a_w_k, a_w_v,
              a_head_norm_g, a_skip, a_pool_w, a_pool_scale, a_w_out, c_norm_g, c_w_in,
              c_q_norm_g, c_kv_norm_g, c_w_uq, c_w_ukv, c_qn_g, c_kn_g, c_w_out):
    for layer in range(DEPTH):
        j = layer // 2
        if layer % 2 == 0:
            x = mlstm_pool_layer(x, a_norm_g[j], a_w_in[j], a_b_if[j], a_conv_w[j], a_conv_b[j],
                                 a_w_q[j], a_w_k[j], a_w_v[j], a_head_norm_g[j], a_skip[j],
                                 a_pool_w[j], a_pool_scale[j], a_w_out[j])
        else:
            x = mla_layer(x, positions, c_norm_g[j], c_w_in[j], c_q_norm_g[j], c_kv_norm_g[j],
                          c_w_uq[j], c_w_ukv[j], c_qn_g[j], c_kn_g[j], c_w_out[j])
    return x
```

```python
import numpy as np
from contextlib import ExitStack
import concourse.bass as bass
import concourse.mybir as mybir
from concourse.bass_utils import run_bass_kernel_spmd

F32 = mybir.dt.float32
BF16 = mybir.dt.bfloat16
I32 = mybir.dt.int32
ALU = mybir.AluOpType
AF = mybir.ActivationFunctionType
AX = mybir.AxisListType

D = 1024
S = 2048
T = 256
NB = S // T
NCH = T // 64
A_IN = 5128
EPS = 1e-6
PI = float(np.pi)


class StopBuild(Exception):
    pass


import os
DEBUG_STOP = int(os.environ.get("KSTOP", "0"))


def stop_at(n):
    if DEBUG_STOP == n:
        raise StopBuild()


class Buf:
    __slots__ = ("name", "w", "r", "dsem", "dval", "excl")

    def __init__(self, name, excl=False):
        self.name = name
        self.excl = excl
        self.w = None
        self.r = {}
        self.dsem = None
        self.dval = 0


class Ctx:
    def __init__(self, nc, stack):
        self.nc = nc
        self.stack = stack
        self.E = {"pe": nc.tensor, "act": nc.scalar, "dve": nc.vector, "pool": nc.gpsimd, "sp": nc.sync}
        self.sem = {e: stack.enter_context(nc.semaphore("sem_" + e)) for e in ("pe", "act", "dve", "pool")}
        self.semid = {id(v): k for k, v in self.sem.items()}
        self.cnt = {e: 0 for e in self.sem}
        self.seen = {e: {} for e in self.E}
        self.bufs = []
        self.nd = 0

    def buf(self, name, excl=False):
        b = Buf(name, excl)
        self.bufs.append(b)
        return b

    def _wait(self, eng, ev, raw):
        sem, val = ev
        own = self.sem.get(eng)
        if own is sem:
            if eng == "pe" or not raw:
                return
        k = id(sem)
        if self.seen[eng].get(k, 0) >= val:
            return
        self.E[eng].wait_ge(sem, val)
        self.seen[eng][k] = val

    def _deps(self, eng, reads, writes):
        for b in reads:
            if b.w is not None:
                self._wait(eng, b.w, True)
            if b.excl:
                for ev in b.r.values():
                    self._wait(eng, ev, False)
        for b in writes:
            if b.w is not None:
                self._wait(eng, b.w, False)
            for ev in b.r.values():
                self._wait(eng, ev, False)

    def _commit(self, ev, reads, writes):
        k = id(ev[0])
        for b in reads:
            b.r[k] = ev
        for b in writes:
            b.w = ev
            b.r = {}

    def op(self, eng, fn, reads=(), writes=()):
        self._deps(eng, reads, writes)
        ins = fn(self.E[eng])
        self.cnt[eng] += 1
        ins.then_inc(self.sem[eng], 1)
        self._commit((self.sem[eng], self.cnt[eng]), reads, writes)

    def mm(self, out, pairs, reads, writes, start=True, stop=True):
        self._deps("pe", reads, writes)
        n = len(pairs)
        ins = None
        for i, (l, r) in enumerate(pairs):
            ins = self.nc.tensor.matmul(out, lhsT=l, rhs=r, start=(start and i == 0), stop=(stop and i == n - 1))
        self.cnt["pe"] += 1
        ins.then_inc(self.sem["pe"], 1)
        self._commit((self.sem["pe"], self.cnt["pe"]), reads, writes)

    def dma(self, out, in_, reads, writes, sbuf):
        self._deps("sp", reads, writes)
        if sbuf.dsem is None:
            self.nd += 1
            sbuf.dsem = self.stack.enter_context(self.nc.semaphore("dsem%d" % self.nd))
        ins = self.nc.sync.dma_start(out=out, in_=in_)
        sbuf.dval += 16
        ins.then_inc(sbuf.dsem, 16)
        self._commit((sbuf.dsem, sbuf.dval), reads, writes)

    def barrier(self):
        for e in ("act", "dve", "pool", "sp", "pe"):
            for e2 in self.sem:
                if self.cnt[e2] > 0:
                    self._wait(e, (self.sem[e2], self.cnt[e2]), True)
            for b in self.bufs:
                if b.dsem is not None and b.dval > 0:
                    self._wait(e, (b.dsem, b.dval), True)

    def final_wait(self):
        for b in self.bufs:
            if b.dsem is not None:
                self._wait("sp", (b.dsem, b.dval), True)
        for e in self.sem:
            self._wait("sp", (self.sem[e], self.cnt[e]), True)


def interleave(*gens):
    gens = [g for g in gens if g is not None]
    while gens:
        for g in list(gens):
            try:
                next(g)
            except StopIteration:
                gens.remove(g)


def build_program(ns, layers):
    nc = bass.Bass("TRN2", target_bir_lowering=False)

    def din(name, shape, dt=F32):
        return nc.dram_tensor(name, list(shape), dt, kind="ExternalInput").ap()

    xT = din("xT", [ns, D, S])
    pos = din("pos", [ns, S], I32)
    oT = nc.dram_tensor("oT", [ns, D, S], F32, kind="ExternalOutput").ap()
    W = {}
    for j in sorted({j for k, j in layers if k == "a"}):
        W["a", j] = dict(
            win=din("a_win%d" % j, [D, A_IN]), wq=din("a_wq%d" % j, [4, 256, 256]),
            wk=din("a_wk%d" % j, [4, 256, 256]), wv=din("a_wv%d" % j, [4, 256, 256]),
            wpool=din("a_wpool%d" % j, [4, 256, 256]), wout=din("a_wout%d" % j, [2048, D]),
            vec=din("a_vec%d" % j, [128, 72]), bif=din("a_bif%d" % j, [4, 2]))
    for j in sorted({j for k, j in layers if k == "c"}):
        W["c", j] = dict(
            win=din("c_win%d" % j, [D, 1792]), wuq=din("c_wuq%d" % j, [384, 2048]),
            wukv=din("c_wukv%d" % j, [256, 2048]), wout=din("c_wout%d" % j, [D, D]),
            vec=din("c_vec%d" % j, [128, 19]))
    c_ones = din("c_ones", [128, 128])
    c_tri = din("c_tri", [64, 64])
    c_id4 = din("c_id4", [4, 8])
    c_sel = din("c_sel", [4, 512])
    c_freq = din("c_freq", [64, 2])
    c_amask = din("c_amask", [128, 2 * T])
    c_pfix = din("c_pfix", [128, 64])

    with ExitStack() as st:
        cx = Ctx(nc, st)

        def sb(name, shape, dt=F32):
            return st.enter_context(nc.sbuf_tensor(name, list(shape), dt))

        def ps(name, shape, dt=F32):
            return st.enter_context(nc.psum_tensor(name, list(shape), dt))

        onesb = sb("onesb", [128, 128], BF16); b_onesb = cx.buf("onesb")
        tri = sb("tri", [64, 64]); b_tri = cx.buf("tri")
        id4 = sb("id4", [4, 8]); b_id4 = cx.buf("id4")
        sel = sb("sel", [4, 512]); b_sel = cx.buf("sel")
        freq = sb("freq", [64, 2]); b_freq = cx.buf("freq")
        amask = sb("amask", [128, 2 * T], BF16); b_am = cx.buf("am")
        pfix = sb("pfix", [128, 64]); b_pfix = cx.buf("pfix")
        for (t_, b_, src) in ((tri, b_tri, c_tri), (id4, b_id4, c_id4),
                              (sel, b_sel, c_sel), (freq, b_freq, c_freq), (pfix, b_pfix, c_pfix)):
            cx.dma(t_[:], src[:, :], [], [b_], b_)

        WCOLS = 67600
        arena = sb("arena", [128, WCOLS], BF16)
        cur_w = []
        stg = [sb("stg%d" % i, [128, 512]) for i in range(2)]
        b_stg = [cx.buf("stg%d" % i) for i in range(2)]
        cx.dma(stg[0][:, 0:128], c_ones[:, :], [], [b_stg[0]], b_stg[0])
        cx.op("dve", lambda e: e.tensor_copy(out=onesb[:], in_=stg[0][:, 0:128]), [b_stg[0]], [b_onesb])
        cx.dma(stg[1][:, 0:2 * T], c_amask[:, :], [], [b_stg[1]], b_stg[1])
        cx.op("dve", lambda e: e.tensor_copy(out=amask[:], in_=stg[1][:, 0:2 * T]), [b_stg[1]], [b_am])
        vec = sb("vec", [128, 72]); b_vec = cx.buf("vec")
        x32 = [sb("x32_0", [128, 8, T])]
        b_x32 = [cx.buf("x32_0")]
        xn = sb("xn", [128, 8, T], BF16); b_xn = cx.buf("xn")
        rst = sb("rst", [128, T]); b_rst = cx.buf("rst")
        y = sb("y", [128, 16, T], BF16); b_y = cx.buf("y")
        sq = y[:, 0:8, :]; b_sq = b_y

        G = [ps("G%d" % i, [128, 512]) for i in range(4)]
        b_G = [cx.buf("G%d" % i, True) for i in range(4)]
        P4 = [ps("P%d" % i, [128, 512]) for i in range(4)]
        b_P4 = [cx.buf("P%d" % i, True) for i in range(4)]
        gi_ = [0]

        def nextG():
            i = gi_[0] % 4
            gi_[0] += 1
            return G[i], b_G[i]

        rr = [0]

        def rr_eng(opts=("act", "dve", "pool")):
            rr[0] += 1
            return opts[rr[0] % len(opts)]

        stg_i = [0]

        def load_w(dst_ap, src_ap, ncols, scale_ap=None, b_scale=None):
            c0 = 0
            while c0 < ncols:
                n = min(512, ncols - c0)
                i = stg_i[0] % 2
                stg_i[0] += 1
                cx.dma(stg[i][:, 0:n], src_ap[:, c0:c0 + n], [], [b_stg[i]], b_stg[i])
                eng = rr_eng()
                d = dst_ap[:, c0:c0 + n]
                s_ = stg[i][:, 0:n]
                bw = cx.buf("w")
                cur_w.append(bw)
                if scale_ap is None:
                    if eng == "act":
                        cx.op("act", lambda e: e.copy(out=d, in_=s_), [b_stg[i]], [bw])
                    else:
                        cx.op(eng, lambda e: e.tensor_copy(out=d, in_=s_), [b_stg[i]], [bw])
                else:
                    if eng == "act":
                        cx.op("act", lambda e: e.activation(out=d, in_=s_, func=AF.Copy, scale=scale_ap),
                              [b_stg[i], b_scale], [bw])
                    else:
                        cx.op(eng, lambda e: e.tensor_scalar(out=d, in0=s_, scalar1=scale_ap, scalar2=None,
                                                             op0=ALU.mult), [b_stg[i], b_scale], [bw])
                c0 += n

        def rstd_from_ps(ps_ap, b_ps, out_ap, b_out, inv_n, eps):
            cx.op("dve", lambda e: e.tensor_scalar(out=out_ap, in0=ps_ap, scalar1=inv_n, scalar2=eps,
                                                   op0=ALU.mult, op1=ALU.add), [b_ps], [b_out])
            cx.op("act", lambda e: e.activation(out=out_ap, in_=out_ap, func=AF.Ln), [b_out], [b_out])
            cx.op("act", lambda e: e.activation(out=out_ap, in_=out_ap, func=AF.Exp, scale=-0.5), [b_out], [b_out])

        def load_block(src, s, blk, par):
            t0 = blk * T
            src_ap = src[s].rearrange("(kc p) t -> p kc t", p=128)[:, :, t0:t0 + T]
            cx.dma(x32[par][:], src_ap, [b_dram[s][blk]], [b_x32[par]], b_x32[par])

        def norm_block(par):
            cx.op("act", lambda e: e.activation(out=sq[:], in_=x32[par][:], func=AF.Square), [b_x32[par]], [b_sq])
            g, bg = nextG()
            cx.mm(g[:, 0:T], [(onesb[:], sq[:, kc, :]) for kc in range(8)], [b_onesb, b_sq], [bg])
            rstd_from_ps(g[:, 0:T], bg, rst[:], b_rst, 1.0 / D, EPS)
            for kc in range(8):
                cx.op("dve", lambda e: e.tensor_tensor(out=xn[:, kc, :], in0=x32[par][:, kc, :],
                                                                          in1=rst[:], op=ALU.mult),
                      [b_x32[par], b_rst], [b_xn])

        def outproj_store(wout_v, nk, s, blk, par):
            for op_ in range(4):
                g, bg = nextG()
                for o2 in range(2):
                    oc = op_ * 2 + o2
                    cx.mm(g[:, o2 * T:(o2 + 1) * T],
                          [(wout_v[:, kc, oc * 128:(oc + 1) * 128], y[:, kc, :]) for kc in range(nk)],
                          cur_w + [b_y], [bg])
                for o2 in range(2):
                    oc = op_ * 2 + o2
                    cx.op("dve", lambda e: e.tensor_tensor(out=x32[par][:, oc, :], in0=g[:, o2 * T:(o2 + 1) * T],
                                                           in1=x32[par][:, oc, :], op=ALU.add),
                          [bg, b_x32[par]], [b_x32[par]])
            t0 = blk * T
            dst_ap = oT[s].rearrange("(kc p) t -> p kc t", p=128)[:, :, t0:t0 + T]
            cx.dma(dst_ap, x32[par][:], [b_x32[par]], [b_dram[s][blk]], b_x32[par])

        open_lst = []
        b_dram = [[cx.buf("dram%d_%d" % (s, b)) for b in range(NB)] for s in range(ns)]

        def layer_a(j, src):
            w = W["a", j]

            lst = ExitStack()
            open_lst.append(lst)

            def sb(name, shape, dt=F32):
                return lst.enter_context(nc.sbuf_tensor(name + "_a%d" % j, list(shape), dt))

            bif = sb("bif", [4, 2]); b_bif = cx.buf("bif")
            nbif = sb("nbif", [4, 2]); b_nbif = cx.buf("nbif")
            xm = sb("xm", [128, 8, 16 + T], BF16); b_xm = [cx.buf("xm%d" % h) for h in range(4)]
            acc = sb("acc", [128, 2, T]); b_acc = cx.buf("acc")
            flB = acc[:, 0, :]; b_flB = b_acc
            xc = sb("xc", [128, 2, T], BF16); b_xc = cx.buf("xc")
            tom = sb("tom", [128, 2, T], BF16); b_tom = cx.buf("tom")
            szm = sb("szm", [128, 2, T], BF16); b_szm = cx.buf("szm")
            qh = sb("qh", [128, 2, T], BF16); b_qh = cx.buf("qh")
            kh = sb("kh", [128, 2, T], BF16); b_kh = cx.buf("kh")
            vsb = sb("vsb", [64, NCH, 256], BF16); b_vsb = cx.buf("vsb")
            kwsb = sb("kwsb", [64, NCH, 256], BF16); b_kwsb = cx.buf("kwsb")
            sTw = [sb("sTw%d" % i, [64, 64], BF16) for i in range(NCH)]; b_sTw = [cx.buf("sTw%d" % i) for i in range(NCH)]

            Cf = sb("Cf", [128, 4, 2, 256]); b_Cf = [cx.buf("Cf%d" % h) for h in range(4)]
            nf = sb("nf", [128, 4, 2]); b_nf = [cx.buf("nf%d" % h) for h in range(4)]
            Cb = [sb("Cb%d" % i, [128, 2, 256], BF16) for i in range(2)]; b_Cb = [cx.buf("Cb%d" % i) for i in range(2)]
            nsc = sb("nsc", [128, 2]); b_nsc = cx.buf("nsc")
            nBc = [sb("nBc%d" % i, [128, 2, 128], BF16) for i in range(2)]; b_nBc = [cx.buf("nBc%d" % i) for i in range(2)]
            nlf = sb("nlf", [4, T]); b_nlf = cx.buf("nlf")
            nlfT = sb("nlfT", [64, NCH * 4]); b_nlfT = cx.buf("nlfT")
            nb_sb = sb("nb_sb", [4, T]); b_nb = cx.buf("nb")
            a_sb = sb("a_sb", [4, T]); b_a = cx.buf("a")
            gi_sb = a_sb; b_gi = b_a
            Amax = sb("Amax", [4, NCH]); b_Amax = cx.buf("Amax")
            Mc = sb("Mc", [4, NCH]); b_Mc = cx.buf("Mc")
            mprev = sb("mprev", [4, NCH + 1]); b_mprev = cx.buf("mprev")
            w_sb = a_sb; b_w = b_a
            fl_sb = nb_sb; b_fl = b_nb
            wc_sb = sb("wc_sb", [4, NCH]); b_wc = cx.buf("wc")
            wsc = sb("wsc", [64, NCH * 4]); b_wsc = cx.buf("wsc")
            wcB = sb("wcB", [128, 4 * NCH]); b_wcB = cx.buf("wcB")
            den = sb("den", [128, T]); b_den = cx.buf("den")
            hh = sb("hh", [128, 2, T]); b_hh = cx.buf("hh")
            sqh = sb("sqh", [128, 2, T], BF16); b_sqh = cx.buf("sqh")
            rsth = sb("rsth", [128, T]); b_rsth = cx.buf("rsth")
            t2 = den; b_t2 = b_den
            xp = sb("xp", [128, 8, 16 + T], BF16); b_xp = [cx.buf("xp%d" % g) for g in range(4)]
            pA = sb("pA", [128, 2, 16 + T], BF16); b_pA = cx.buf("pA")
            pB = sb("pB", [128, 2, 16 + T], BF16); b_pB = cx.buf("pB")
            mixb = sb("mixb", [128, 2, T], BF16); b_mix = cx.buf("mix")
            szp = sb("szp", [128, 2, T], BF16); b_szp = cx.buf("szp")
            sT_ps, b_sTps = P4[0], b_P4[0]
            NT_ps, b_NT = P4[1], b_P4[1]
            Dn_ps, b_Dn = P4[2], b_P4[2]
            sTb = [(P4[0], b_P4[0]), (P4[3], b_P4[3])]
            cx.barrier()
            del cur_w[:]
            cx.dma(vec[:, 0:72], w["vec"][:, :], [], [b_vec], b_vec)
            cx.dma(bif[:], w["bif"][:, :], [], [b_bif], b_bif)
            cx.op("dve", lambda e: e.tensor_scalar(out=nbif[:], in0=bif[:], scalar1=-1.0, scalar2=None, op0=ALU.mult),
                  [b_bif], [b_nbif])
            o = 0
            win_v = arena[:, o:o + 8 * A_IN].rearrange("p (k n) -> p k n", k=8); o += 8 * A_IN
            wq_v = arena[:, o:o + 2048].rearrange("p (h c n) -> p h c n", h=4, c=2); o += 2048
            wk_v = arena[:, o:o + 2048].rearrange("p (h c n) -> p h c n", h=4, c=2); o += 2048
            wv_v = arena[:, o:o + 2048].rearrange("p (h c n) -> p h c n", h=4, c=2); o += 2048
            wp_v = arena[:, o:o + 2048].rearrange("p (h c n) -> p h c n", h=4, c=2); o += 2048
            wo_v = arena[:, o:o + 16 * D].rearrange("p (k n) -> p k n", k=16); o += 16 * D
            assert o <= WCOLS
            win_src = w["win"].rearrange("(k p) n -> p k n", p=128)
            for kc in range(8):
                load_w(win_v[:, kc, :], win_src[:, kc, :], A_IN, vec[:, kc:kc + 1], b_vec)
            for (dv, nm) in ((wq_v, "wq"), (wk_v, "wk"), (wv_v, "wv"), (wp_v, "wpool")):
                srcw = w[nm].rearrange("h (c p) n -> p h c n", p=128)
                for h in range(4):
                    for c in range(2):
                        load_w(dv[:, h, c, :], srcw[:, h, c, :], 256)
            wo_src = w["wout"].rearrange("(k p) n -> p k n", p=128)
            for kc in range(16):
                load_w(wo_v[:, kc, :], wo_src[:, kc, :], D)

            V_CW, V_CB, V_HNG, V_SKIP, V_PS = 8, 40, 48, 56, 64
            stop_at(1)

            def inproj2(col0):
                g, bg = nextG()
                for oc in range(2):
                    c0 = col0 + oc * 128
                    cx.mm(g[:, oc * T:(oc + 1) * T],
                          [(win_v[:, kc, c0:c0 + 128], xn[:, kc, :]) for kc in range(8)], cur_w + [b_xn], [bg])
                return g[:, 0:2 * T].rearrange("p (a t) -> p a t", a=2), bg

            for s in range(ns):
                cx.op("pool", lambda e: e.memset(Cf[:], 0.0), [], b_Cf)
                cx.op("pool", lambda e: e.memset(nf[:], 0.0), [], b_nf)
                cx.op("pool", lambda e: e.memset(xm[:], 0.0), [], b_xm)
                cx.op("pool", lambda e: e.memset(xp[:], 0.0), [], b_xp)
                cx.op("pool", lambda e: e.memset(mprev[:], 0.0), [], [b_mprev])
                for blk in range(NB):
                    par = 0
                    load_block(src, s, blk, par)
                    norm_block(par)
                    stop_at(2)
                    g, bg = nextG()
                    cx.mm(g[0:4, 0:T], [(win_v[:, kc, 2048:2052], xn[:, kc, :]) for kc in range(8)], cur_w + [b_xn], [bg])
                    cx.mm(g[0:4, T:2 * T], [(win_v[:, kc, 2052:2056], xn[:, kc, :]) for kc in range(8)], cur_w + [b_xn], [bg])
                    cx.op("act", lambda e: e.activation(out=gi_sb[:], in_=g[0:4, 0:T], func=AF.Identity, bias=bif[:, 0:1]),
                          [bg, b_bif], [b_gi])
                    cx.op("act", lambda e: e.activation(out=nlf[:], in_=g[0:4, T:2 * T], func=AF.Exp, scale=-1.0,
                                                        bias=nbif[:, 1:2]), [bg, b_nbif], [b_nlf])
                    cx.op("act", lambda e: e.activation(out=nlf[:], in_=nlf[:], func=AF.Ln, bias=1.0), [b_nlf], [b_nlf])
                    g2, bg2 = nextG()
                    for c in range(NCH):
                        cx.mm(g2[0:64, c * 4:(c + 1) * 4], [(nlf[0:4, c * 64:(c + 1) * 64], id4[0:4, 0:4])],
                              [b_nlf, b_id4], [bg2])
                    cx.op("act", lambda e: e.copy(out=nlfT[:], in_=g2[0:64, 0:NCH * 4]), [bg2], [b_nlfT])
                    g3, bg3 = nextG()
                    for c in range(NCH):
                        cx.mm(g3[0:4, c * 64:(c + 1) * 64], [(nlfT[:, c * 4:(c + 1) * 4], tri[:])], [b_nlfT, b_tri], [bg3])
                    cx.op("act", lambda e: e.copy(out=nb_sb[:], in_=g3[0:4, 0:T]), [bg3], [b_nb])
                    cx.op("dve", lambda e: e.tensor_tensor(out=a_sb[:], in0=a_sb[:], in1=nb_sb[:], op=ALU.add),
                          [b_a, b_nb], [b_a])
                    cx.op("dve", lambda e: e.tensor_reduce(out=Amax[:], in_=a_sb[:].rearrange("p (c t) -> p c t", c=NCH),
                                                           axis=AX.X, op=ALU.max), [b_a], [b_Amax])
                    cx.op("dve", lambda e: e.tensor_copy(out=mprev[:, 0:1], in_=mprev[:, NCH:NCH + 1]), [b_mprev], [b_mprev])
                    for c in range(NCH):
                        cx.op("dve", lambda e: e.tensor_tensor(out=Mc[:, c:c + 1], in0=mprev[:, c:c + 1],
                                                               in1=Amax[:, c:c + 1], op=ALU.max),
                              [b_mprev, b_Amax], [b_Mc])
                        cx.op("dve", lambda e: e.tensor_tensor(out=mprev[:, c + 1:c + 2], in0=Mc[:, c:c + 1],
                                                               in1=nb_sb[:, c * 64 + 63:c * 64 + 64], op=ALU.subtract),
                              [b_Mc, b_nb], [b_mprev])
                    for c in range(NCH):
                        cs = slice(c * 64, (c + 1) * 64)
                        cx.op("dve", lambda e: e.tensor_scalar(out=a_sb[:, cs], in0=a_sb[:, cs], scalar1=Mc[:, c:c + 1],
                                                               scalar2=None, op0=ALU.subtract), [b_a, b_Mc], [b_a])
                        cx.op("dve", lambda e: e.tensor_scalar(out=nb_sb[:, cs], in0=nb_sb[:, cs], scalar1=Mc[:, c:c + 1],
                                                               scalar2=None, op0=ALU.subtract), [b_nb, b_Mc], [b_nb])
                    cx.op("dve", lambda e: e.tensor_tensor(out=wc_sb[:], in0=mprev[:, 0:NCH], in1=Mc[:], op=ALU.subtract),
                          [b_mprev, b_Mc], [b_wc])
                    cx.op("act", lambda e: e.activation(out=w_sb[:], in_=a_sb[:], func=AF.Exp), [b_a], [b_w])
                    cx.op("act", lambda e: e.activation(out=fl_sb[:], in_=nb_sb[:], func=AF.Exp), [b_nb], [b_fl])
                    cx.op("act", lambda e: e.activation(out=wc_sb[:], in_=wc_sb[:], func=AF.Exp), [b_wc], [b_wc])
                    g4, bg4 = nextG()
                    for c in range(NCH):
                        cx.mm(g4[0:64, c * 4:(c + 1) * 4], [(w_sb[0:4, c * 64:(c + 1) * 64], id4[0:4, 4:8])],
                              [b_w, b_id4], [bg4])
                    cx.op("act", lambda e: e.copy(out=wsc[:], in_=g4[0:64, 0:NCH * 4]), [bg4], [b_wsc])
                    g5, bg5 = nextG()
                    for h in range(4):
                        cx.mm(g5[:, h * NCH:(h + 1) * NCH], [(sel[0:4, h * 128:(h + 1) * 128], wc_sb[:])], [b_sel, b_wc], [bg5])
                    cx.op("dve", lambda e: e.tensor_copy(out=wcB[:], in_=g5[:, 0:4 * NCH]), [bg5], [b_wcB])
                    stop_at(3)
                    def head_gen(h):
                        pv, bp = inproj2(h * 256)
                        xmh = xm[:, 2 * h:2 * h + 2, :]
                        cx.op("act", lambda e: e.copy(out=xmh[:, :, 16:16 + T], in_=pv), [bp], [b_xm[h]])
                        for oc in range(2):
                            ch = 2 * h + oc
                            for k in range(4):
                                src_k = xm[:, ch, 13 + k:13 + k + T]
                                cwk = vec[:, V_CW + k * 8 + ch:V_CW + k * 8 + ch + 1]
                                if k == 0:
                                    cx.op("dve", lambda e: e.tensor_scalar(out=acc[:, oc, :], in0=src_k, scalar1=cwk,
                                                                            scalar2=None, op0=ALU.mult),
                                          [b_xm[h], b_vec], [b_acc])
                                else:
                                    cx.op("dve", lambda e: e.scalar_tensor_tensor(out=acc[:, oc, :], in0=src_k, scalar=cwk,
                                                                                   in1=acc[:, oc, :], op0=ALU.mult,
                                                                                   op1=ALU.add),
                                          [b_xm[h], b_vec, b_acc], [b_acc])
                            cx.op("act", lambda e: e.activation(out=xc[:, oc, :], in_=acc[:, oc, :], func=AF.Silu,
                                                                bias=vec[:, V_CB + ch:V_CB + ch + 1]),
                                  [b_acc, b_vec], [b_xc])
                        cx.op("pool", lambda e: e.tensor_copy(out=xmh[:, :, 0:16], in_=xmh[:, :, T:T + 16]),
                              [b_xm[h]], [b_xm[h]])
                        stop_at(7)
                        yield
                        pv, bp = inproj2(1024 + h * 256)
                        cx.op("act", lambda e: e.activation(out=tom[:], in_=pv, func=AF.Tanh, scale=0.5), [bp], [b_tom])
                        pv, bp = inproj2(2056 + h * 256)
                        cx.op("act", lambda e: e.activation(out=szm[:], in_=pv, func=AF.Silu), [bp], [b_szm])
                        stop_at(8)
                        yield
                        for (wv_, dst, bd, eng) in ((wq_v, qh, b_qh, "dve"), (wk_v, kh, b_kh, "act")):
                            g, bg = nextG()
                            for oc in range(2):
                                cx.mm(g[:, oc * T:(oc + 1) * T],
                                      [(wv_[:, h, cc, oc * 128:(oc + 1) * 128], xc[:, cc, :]) for cc in range(2)],
                                      cur_w + [b_xc], [bg])
                            pvv = g[:, 0:2 * T].rearrange("p (a t) -> p a t", a=2)
                            if eng == "dve":
                                cx.op("dve", lambda e: e.tensor_copy(out=dst[:], in_=pvv), [bg], [bd])
                            else:
                                cx.op("act", lambda e: e.copy(out=dst[:], in_=pvv), [bg], [bd])
                        stop_at(9)
                        yield
                        for c in range(NCH):
                            cs = slice(c * 64, (c + 1) * 64)
                            g, bg = nextG()
                            cx.mm(g[0:64, 0:256], [(xm[:, 2 * h + cc, 16 + c * 64:16 + (c + 1) * 64], wv_v[:, h, cc, :]) for cc in range(2)],
                                  cur_w + [b_xm[h]], [bg])
                            stop_at(14)
                            cx.mm(g[0:64, 256:512], [(xc[:, cc, cs], wk_v[:, h, cc, :]) for cc in range(2)],
                                  cur_w + [b_xc], [bg])
                            stop_at(15)
                            cx.op("act", lambda e: e.copy(out=vsb[:, c, 0:256], in_=g[0:64, 0:256]), [bg], [b_vsb])
                            stop_at(16)
                            cx.op("act", lambda e: e.activation(out=kwsb[:, c, :], in_=g[0:64, 256:512], func=AF.Copy,
                                                                scale=wsc[:, c * 4 + h:c * 4 + h + 1]), [bg, b_wsc], [b_kwsb])
                        stop_at(10)
                        yield
                        Ug = []
                        for c in range(NCH):
                            cs = slice(c * 64, (c + 1) * 64)
                            sTp, b_sTp = sTb[c % 2]
                            s0 = (c // 2) * 64
                            cx.mm(sTp[0:64, s0:s0 + 64], [(kh[:, dc, cs], qh[:, dc, cs]) for dc in range(2)],
                                  [b_kh, b_qh], [b_sTp])
                            gU, bU = nextG()
                            for dc in range(2):
                                cx.mm(gU[:, dc * 256:(dc + 1) * 256],
                                      [(kwsb[:, c, dc * 128:(dc + 1) * 128], vsb[:, c, 0:256])], [b_kwsb, b_vsb], [bU])
                                cx.mm(sTp[:, 256 + 2 * c + dc:256 + 2 * c + dc + 1],
                                      [(kwsb[:, c, dc * 128:(dc + 1) * 128], onesb[0:64, 0:1])], [b_kwsb, b_onesb], [b_sTp])
                            cx.op("dve", lambda e: e.scalar_tensor_tensor(out=sTw[c][:], in0=sTp[0:64, s0:s0 + 64],
                                                                          scalar=wsc[:, c * 4 + h:c * 4 + h + 1], in1=tri[:],
                                                                          op0=ALU.mult, op1=ALU.mult),
                                  [b_sTp, b_wsc, b_tri], [b_sTw[c]])
                            Ug.append((gU, bU))
                        for c in range(NCH):
                            cs = slice(c * 64, (c + 1) * 64)
                            ci = c % 2
                            wcol = wcB[:, h * NCH + c:h * NCH + c + 1]
                            cx.op("act", lambda e: e.activation(out=Cb[ci][:], in_=Cf[:, h], func=AF.Copy, scale=wcol),
                                  [b_Cf[h], b_wcB], [b_Cb[ci]])
                            cx.op("dve", lambda e: e.tensor_scalar(out=nsc[:], in0=nf[:, h, :], scalar1=wcol, scalar2=None,
                                                                   op0=ALU.mult), [b_nf[h], b_wcB], [b_nsc])
                            for dc in range(2):
                                cx.op("dve", lambda e: e.tensor_scalar(out=nBc[ci][:, dc, :], in0=onesb[:],
                                                                       scalar1=nsc[:, dc:dc + 1], scalar2=None,
                                                                       op0=ALU.mult), [b_onesb, b_nsc], [b_nBc[ci]])
                            gU, bU = Ug[c]
                            cx.op("dve", lambda e: e.scalar_tensor_tensor(
                                out=Cf[:, h], in0=Cf[:, h], scalar=wcol,
                                in1=gU[:, 0:512].rearrange("p (a n) -> p a n", a=2), op0=ALU.mult, op1=ALU.add),
                                [b_Cf[h], b_wcB, bU], [b_Cf[h]])
                            cx.op("dve", lambda e: e.scalar_tensor_tensor(
                                out=nf[:, h, :], in0=nf[:, h, :], scalar=wcol, in1=sTb[c % 2][0][:, 256 + 2 * c:256 + 2 * c + 2],
                                op0=ALU.mult, op1=ALU.add), [b_nf[h], b_wcB, sTb[c % 2][1]], [b_nf[h]])
                            for jv in range(2):
                                cx.mm(NT_ps[:, jv * T + c * 64:jv * T + (c + 1) * 64],
                                      [(vsb[:, c, jv * 128:(jv + 1) * 128], sTw[c][:])] +
                                      [(Cb[ci][:, dc, jv * 128:(jv + 1) * 128], qh[:, dc, cs]) for dc in range(2)],
                                      [b_vsb, b_sTw[c], b_Cb[ci], b_qh], [b_NT])
                            cx.mm(Dn_ps[:, cs], [(onesb[0:64, :], sTw[c][:])] +
                                  [(nBc[ci][:, dc, :], qh[:, dc, cs]) for dc in range(2)],
                                  [b_onesb, b_sTw[c], b_nBc[ci], b_qh], [b_Dn])
                        stop_at(12)
                        yield
                        g5, bg5 = nextG()
                        cx.mm(g5[:, 0:T], [(sel[0:4, h * 128:(h + 1) * 128], fl_sb[:])], [b_sel, b_fl], [bg5])
                        cx.op("act", lambda e: e.copy(out=flB, in_=g5[:, 0:T]), [bg5], [b_flB])
                        cx.op("act", lambda e: e.activation(out=den[:], in_=Dn_ps[:, 0:T], func=AF.Abs), [b_Dn], [b_den])
                        cx.op("dve", lambda e: e.tensor_tensor(out=den[:], in0=den[:], in1=flB,
                                                               op=ALU.max), [b_den, b_flB], [b_den])
                        cx.op("act", lambda e: e.activation(out=den[:], in_=den[:], func=AF.Ln), [b_den], [b_den])
                        cx.op("act", lambda e: e.activation(out=den[:], in_=den[:], func=AF.Exp, scale=-1.0), [b_den], [b_den])
                        for jv in range(2):
                            cx.op("dve", lambda e: e.tensor_tensor(out=hh[:, jv, :], in0=NT_ps[:, jv * T:(jv + 1) * T],
                                                                   in1=den[:], op=ALU.mult), [b_NT, b_den], [b_hh])
                        cx.op("dve", lambda e: e.scalar_tensor_tensor(out=hh[:], in0=tom[:], scalar=1.0, in1=hh[:],
                                                                       op0=ALU.add, op1=ALU.mult), [b_tom, b_hh], [b_hh])
                        cx.op("act", lambda e: e.activation(out=sqh[:], in_=hh[:], func=AF.Square), [b_hh], [b_sqh])
                        g, bg = nextG()
                        cx.mm(g[:, 0:T], [(onesb[:], sqh[:, jv, :]) for jv in range(2)], [b_onesb, b_sqh], [bg])
                        rstd_from_ps(g[:, 0:T], bg, rsth[:], b_rsth, 1.0 / 256, 4.0 * EPS)
                        yield
                        for jv in range(2):
                            ch = 2 * h + jv
                            cx.op("dve", lambda e: e.scalar_tensor_tensor(out=t2[:], in0=hh[:, jv, :],
                                                                          scalar=vec[:, V_HNG + ch:V_HNG + ch + 1],
                                                                          in1=rsth[:], op0=ALU.mult, op1=ALU.mult),
                                  [b_hh, b_vec, b_rsth], [b_t2])
                            cx.op("dve", lambda e: e.scalar_tensor_tensor(out=t2[:], in0=xc[:, jv, :],
                                                                           scalar=vec[:, V_SKIP + ch:V_SKIP + ch + 1],
                                                                           in1=t2[:], op0=ALU.mult, op1=ALU.add),
                                  [b_xc, b_vec, b_t2], [b_t2])
                            cx.op("dve", lambda e: e.tensor_tensor(out=y[:, ch, :], in0=t2[:], in1=szm[:, jv, :],
                                                                    op=ALU.mult), [b_t2, b_szm], [b_y])
                    stop_at(4)
                    def pool_gen(gq):
                        wlog = gq + 1
                        pv, bp = inproj2(3080 + gq * 256)
                        xpg = xp[:, 2 * gq:2 * gq + 2, :]
                        cx.op("act", lambda e: e.copy(out=xpg[:, :, 16:16 + T], in_=pv), [bp], [b_xp[gq]])
                        yield
                        pv, bp = inproj2(4104 + gq * 256)
                        cx.op("act", lambda e: e.activation(out=szp[:], in_=pv, func=AF.Silu), [bp], [b_szp])
                        yield
                        cur, bcur = xpg, b_xp[gq]
                        tgl = [(pA, b_pA), (pB, b_pB)]
                        for st_ in range(wlog):
                            sh = 1 << st_
                            lo = 2 * sh
                            dst, bdst = tgl[st_ % 2]
                            cx.op("pool", lambda e: e.tensor_tensor(out=dst[:, :, lo:16 + T], in0=cur[:, :, lo:16 + T],
                                                                    in1=cur[:, :, lo - sh:16 + T - sh], op=ALU.add),
                                  [bcur], [bdst])
                            cur, bcur = dst, bdst
                        if blk == 0:
                            cx.op("pool", lambda e: e.tensor_tensor(
                                out=cur[:, :, 16:32], in0=cur[:, :, 16:32],
                                in1=pfix[:, gq * 16:(gq + 1) * 16].unsqueeze(1).to_broadcast([128, 2, 16]), op=ALU.mult),
                                [bcur, b_pfix], [bcur])
                        cx.op("dve", lambda e: e.scalar_tensor_tensor(out=mixb[:], in0=cur[:, :, 16:16 + T],
                                                                      scalar=1.0 / (1 << wlog), in1=xpg[:, :, 16:16 + T],
                                                                      op0=ALU.mult, op1=ALU.subtract),
                              [bcur, b_xp[gq]], [b_mix])
                        cx.op("pool", lambda e: e.tensor_copy(out=xpg[:, :, 0:16], in_=xpg[:, :, T:T + 16]),
                              [b_xp[gq]], [b_xp[gq]])
                        yield
                        g, bg = nextG()
                        for oc in range(2):
                            cx.mm(g[:, oc * T:(oc + 1) * T],
                                  [(wp_v[:, gq, cc, oc * 128:(oc + 1) * 128], mixb[:, cc, :]) for cc in range(2)],
                                  cur_w + [b_mix], [bg])
                        for oc in range(2):
                            ch = 2 * gq + oc
                            cx.op("dve", lambda e: e.scalar_tensor_tensor(out=y[:, 8 + ch, :], in0=g[:, oc * T:(oc + 1) * T],
                                                                          scalar=vec[:, V_PS + ch:V_PS + ch + 1],
                                                                          in1=szp[:, oc, :], op0=ALU.mult, op1=ALU.mult),
                                  [bg, b_vec, b_szp], [b_y])
                    for h_ in range(4):
                        interleave(head_gen(h_), pool_gen(h_))
                    stop_at(5)
                    outproj_store(wo_v, 16, s, blk, par)
                    stop_at(6)
            cx.barrier()
            lst.close()

        def layer_c(j, src):
            w = W["c", j]
            lst = ExitStack()
            open_lst.append(lst)

            def sb(name, shape, dt=F32):
                return lst.enter_context(nc.sbuf_tensor(name + "_c%d" % j, list(shape), dt))

            cvec = sb("cvec", [128, 4]); b_cvec = cx.buf("cvec")
            posi = sb("posi", [64, T], I32); b_posi = cx.buf("posi")
            ua = sb("ua", [64, T]); b_ua = cx.buf("ua")
            ub = sb("ub", [64, T]); b_ub = cx.buf("ub")
            ki = sb("ki", [64, T], I32); b_ki = cx.buf("ki")
            tA = sb("tA", [64, T]); b_tA = cx.buf("tA")
            tB = sb("tB", [64, T]); b_tB = cx.buf("tB")
            sinT = sb("sinT", [64, T]); b_sin = cx.buf("sin")
            cosT = sb("cosT", [64, T]); b_cos = cx.buf("cos")
            cq32 = sb("cq32", [128, 3, T]); b_cq32 = cx.buf("cq32")
            cqn = sb("cqn", [128, 3, T], BF16); b_cqn = cx.buf("cqn")
            ckvn = sb("ckvn", [128, 2, T], BF16); b_ckvn = cx.buf("ckvn")
            sqc = sb("sqc", [128, 3, T], BF16); b_sqc = cx.buf("sqc")
            rsc = sb("rsc", [128, T]); b_rsc = cx.buf("rsc")
            sqkp = sb("sqkp", [64, T], BF16); b_sqkp = cx.buf("sqkp")
            sqk = sb("sqk", [128, 2, T], BF16); b_sqk = cx.buf("sqk")
            kss = sb("kss", [128, 16]); b_kss = cx.buf("kss")
            kscale = sb("kscale", [128, 16, 8]); b_ksc = cx.buf("kscale")
            sqq = sb("sqq", [128, T], BF16); b_sqq = cx.buf("sqq")
            sqqr = sb("sqqr", [64, T], BF16); b_sqqr = cx.buf("sqqr")
            rq = sb("rq", [128, T]); b_rq = cx.buf("rq")
            qn = [sb("qn%d" % i, [128, T], BF16) for i in range(2)]; b_qn = [cx.buf("qn%d" % i) for i in range(2)]
            qr = [sb("qr%d" % i, [64, T], BF16) for i in range(2)]; b_qr = [cx.buf("qr%d" % i) for i in range(2)]
            qtmp = sb("qtmp", [128, T]); b_qtmp = cx.buf("qtmp")
            szall = sb("szall", [128, 8, T], BF16); b_sz = cx.buf("sz")
            Pt = [sb("Pt%d" % i, [128, T], BF16) for i in range(2)]; b_Pt = [cx.buf("Pt%d" % i) for i in range(2)]
            rinv = sb("rinv", [128, T]); b_rinv = cx.buf("rinv")
            ob = sb("ob", [128, T]); b_ob = cx.buf("ob")
            O_ps, b_O = P4[0], b_P4[0]
            R_ps, b_R = P4[1], b_P4[1]
            K_ps, b_K = P4[2], b_P4[2]
            Sb = [(P4[2], b_P4[2]), (P4[3], b_P4[3])]
            cx.barrier()
            del cur_w[:]
            cx.dma(vec[:, 0:19], w["vec"][:, :], [], [b_vec], b_vec)
            cx.op("dve", lambda e: e.tensor_tensor(out=cvec[:, 0:1], in0=vec[:, 13:14], in1=vec[:, 14:15], op=ALU.mult),
                  [b_vec], [b_cvec])
            cx.op("dve", lambda e: e.tensor_tensor(out=cvec[0:64, 1:2], in0=vec[0:64, 16:17], in1=freq[:, 1:2], op=ALU.mult),
                  [b_vec, b_freq], [b_cvec])
            cx.op("dve", lambda e: e.tensor_tensor(out=cvec[0:64, 2:3], in0=vec[0:64, 18:19], in1=freq[:, 1:2], op=ALU.mult),
                  [b_vec, b_freq], [b_cvec])
            o = 0
            win_v = arena[:, o:o + 8 * 1792].rearrange("p (k n) -> p k n", k=8); o += 8 * 1792
            wuq_v = arena[:, o:o + 3 * 2048].rearrange("p (k n) -> p k n", k=3); o += 3 * 2048
            wukv_v = arena[:, o:o + 2 * 2048].rearrange("p (k n) -> p k n", k=2); o += 2 * 2048
            wo_v = arena[:, o:o + 8 * D].rearrange("p (k n) -> p k n", k=8); o += 8 * D
            KT = arena[:, o:o + 8 * S].rearrange("p (h t) -> p h t", h=8); o += 8 * S
            V = arena[:, o:o + 16 * 1024].rearrange("p (j n) -> p j n", j=16); o += 16 * 1024
            kr = arena[:, o:o + S]; o += S
            assert o <= WCOLS
            b_KT = cx.buf("KT"); b_V = cx.buf("V"); b_kr = cx.buf("kr")
            win_src = w["win"].rearrange("(k p) n -> p k n", p=128)
            for kc in range(8):
                load_w(win_v[:, kc, :], win_src[:, kc, :], 1792, vec[:, kc:kc + 1], b_vec)
            wuq_src = w["wuq"].rearrange("(k p) n -> p k n", p=128)
            for kc in range(3):
                load_w(wuq_v[:, kc, :], wuq_src[:, kc, :], 2048, vec[:, 8 + kc:9 + kc], b_vec)
            wukv_src = w["wukv"].rearrange("(k p) n -> p k n", p=128)
            for kc in range(2):
                load_w(wukv_v[:, kc, :], wukv_src[:, kc, :], 2048, vec[:, 11 + kc:12 + kc], b_vec)
            wo_src = w["wout"].rearrange("(k p) n -> p k n", p=128)
            for kc in range(8):
                load_w(wo_v[:, kc, :], wo_src[:, kc, :], D)

            def inp(dst_ps, col0, m, bg):
                cx.mm(dst_ps, [(win_v[:, kc, col0:col0 + m], xn[:, kc, :]) for kc in range(8)], cur_w + [b_xn], [bg])

            for s in range(ns):
                for blk in range(NB):
                    par = 0
                    t0 = blk * T
                    load_block(src, s, blk, par)
                    norm_block(par)
                    stop_at(21)
                    pos_ap = bass.AP(pos.tensor, s * S + t0, [[0, 64], [1, T]])
                    cx.dma(posi[:], pos_ap, [], [b_posi], b_posi)
                    cx.op("dve", lambda e: e.tensor_copy(out=ua[:], in_=posi[:]), [b_posi], [b_ua])
                    cx.op("dve", lambda e: e.tensor_scalar(out=ua[:], in0=ua[:], scalar1=freq[:, 0:1], scalar2=None,
                                                           op0=ALU.mult), [b_ua, b_freq], [b_ua])
                    cx.op("dve", lambda e: e.tensor_scalar(out=ub[:], in0=ua[:], scalar1=0.25, scalar2=None, op0=ALU.add),
                          [b_ua], [b_ub])
                    for (su, bsu, dst, bdst) in ((ua, b_ua, sinT, b_sin), (ub, b_ub, cosT, b_cos)):
                        cx.op("dve", lambda e: e.tensor_copy(out=ki[:], in_=su[:]), [bsu], [b_ki])
                        cx.op("dve", lambda e: e.tensor_copy(out=tA[:], in_=ki[:]), [b_ki], [b_tA])
                        cx.op("dve", lambda e: e.tensor_tensor(out=tB[:], in0=su[:], in1=tA[:], op=ALU.subtract),
                              [bsu, b_tA], [b_tB])
                        cx.op("act", lambda e: e.activation(out=dst[:], in_=tB[:], func=AF.Sin, scale=2.0 * PI),
                              [b_tB], [bdst])
                    stop_at(22)
                    for hp in range(4):
                        g, bg = nextG()
                        for i in range(2):
                            inp(g[:, i * T:(i + 1) * T], 704 + (2 * hp + i) * 128, 128, bg)
                        cx.op("act", lambda e: e.activation(out=szall[:, 2 * hp:2 * hp + 2, :],
                                                            in_=g[:, 0:2 * T].rearrange("p (a t) -> p a t", a=2),
                                                            func=AF.Silu), [bg], [b_sz])
                    stop_at(23)
                    g, bg = nextG()
                    inp(g[:, 0:T], 0, 128, bg)
                    inp(g[:, T:2 * T], 128, 128, bg)
                    g2, bg2 = nextG()
                    inp(g2[:, 0:T], 256, 128, bg2)
                    cx.op("act", lambda e: e.copy(out=cq32[:, 0:2, :], in_=g[:, 0:2 * T].rearrange("p (a t) -> p a t", a=2)),
                          [bg], [b_cq32])
                    cx.op("act", lambda e: e.copy(out=cq32[:, 2, :], in_=g2[:, 0:T]), [bg2], [b_cq32])
                    cx.op("act", lambda e: e.activation(out=sqc[:], in_=cq32[:], func=AF.Square), [b_cq32], [b_sqc])
                    g3, bg3 = nextG()
                    cx.mm(g3[:, 0:T], [(onesb[:], sqc[:, kc, :]) for kc in range(3)], [b_onesb, b_sqc], [bg3])
                    rstd_from_ps(g3[:, 0:T], bg3, rsc[:], b_rsc, 1.0 / 384, EPS)
                    for kc in range(3):
                        cx.op("dve", lambda e: e.tensor_tensor(out=cqn[:, kc, :], in0=cq32[:, kc, :],
                                                                                  in1=rsc[:], op=ALU.mult),
                              [b_cq32, b_rsc], [b_cqn])
                    g, bg = nextG()
                    inp(g[:, 0:T], 384, 128, bg)
                    inp(g[:, T:2 * T], 512, 128, bg)
                    cx.op("act", lambda e: e.copy(out=cq32[:, 0:2, :], in_=g[:, 0:2 * T].rearrange("p (a t) -> p a t", a=2)),
                          [bg], [b_cq32])
                    cx.op("act", lambda e: e.activation(out=sqc[:, 0:2, :], in_=cq32[:, 0:2, :], func=AF.Square),
                          [b_cq32], [b_sqc])
                    g3, bg3 = nextG()
                    cx.mm(g3[:, 0:T], [(onesb[:], sqc[:, kc, :]) for kc in range(2)], [b_onesb, b_sqc], [bg3])
                    rstd_from_ps(g3[:, 0:T], bg3, rsc[:], b_rsc, 1.0 / 256, EPS)
                    for kc in range(2):
                        cx.op("dve", lambda e: e.tensor_tensor(out=ckvn[:, kc, :], in0=cq32[:, kc, :],
                                                                                  in1=rsc[:], op=ALU.mult),
                              [b_cq32, b_rsc], [b_ckvn])
                    stop_at(24)
                    g, bg = nextG()
                    inp(g[0:64, 0:T], 640, 64, bg)
                    inp(g[0:64, T:2 * T], 1728, 64, bg)
                    stop_at(31)
                    cx.op("act", lambda e: e.copy(out=ua[:], in_=g[0:64, 0:T]), [bg], [b_ua])
                    cx.op("act", lambda e: e.copy(out=ub[:], in_=g[0:64, T:2 * T]), [bg], [b_ub])
                    cx.op("dve", lambda e: e.scalar_tensor_tensor(out=tA[:], in0=ua[:], scalar=vec[0:64, 17:18],
                                                                  in1=cosT[:], op0=ALU.mult, op1=ALU.mult),
                          [b_ua, b_vec, b_cos], [b_tA])
                    stop_at(32)
                    cx.op("dve", lambda e: e.scalar_tensor_tensor(out=tB[:], in0=ub[:], scalar=cvec[0:64, 2:3],
                                                                  in1=sinT[:], op0=ALU.mult, op1=ALU.mult),
                          [b_ub, b_cvec, b_sin], [b_tB])
                    stop_at(33)
                    cx.op("dve", lambda e: e.tensor_tensor(out=kr[0:64, t0:t0 + T], in0=tA[:], in1=tB[:], op=ALU.add),
                          [b_tA, b_tB], [b_kr])
                    stop_at(34)
                    cx.op("act", lambda e: e.activation(out=sqkp[:], in_=ua[:], func=AF.Square), [b_ua], [b_sqkp])
                    stop_at(25)
                    for hp in range(4):
                        g, bg = nextG()
                        for i in range(2):
                            h = 2 * hp + i
                            cx.mm(g[:, i * T:(i + 1) * T],
                                  [(wukv_v[:, kc, h * 128:(h + 1) * 128], ckvn[:, kc, :]) for kc in range(2)],
                                  cur_w + [b_ckvn], [bg])
                        gv = g[:, 0:2 * T].rearrange("p (a t) -> p a t", a=2)
                        cx.op("act", lambda e: e.copy(out=KT[:, 2 * hp:2 * hp + 2, t0:t0 + T], in_=gv), [bg], [b_KT])
                        cx.op("act", lambda e: e.activation(out=sqk[:], in_=gv, func=AF.Square), [bg], [b_sqk])
                        for st2 in range(2):
                            for i in range(2):
                                h = 2 * hp + i
                                cx.mm(K_ps[:, st2 * 8 + h:st2 * 8 + h + 1],
                                      [(sqk[:, i, st2 * 128:(st2 + 1) * 128], onesb[:, 0:1]),
                                       (sqkp[0:64, st2 * 128:(st2 + 1) * 128], onesb[0:64, 0:1])],
                                      [b_sqk, b_sqkp, b_onesb], [b_K])
                    cx.op("dve", lambda e: e.tensor_scalar(out=kss[:], in0=K_ps[:, 0:16], scalar1=1.0 / 192, scalar2=EPS,
                                                           op0=ALU.mult, op1=ALU.add), [b_K], [b_kss])
                    cx.op("act", lambda e: e.activation(out=kss[:], in_=kss[:], func=AF.Ln), [b_kss], [b_kss])
                    cx.op("act", lambda e: e.activation(out=kss[:], in_=kss[:], func=AF.Exp, scale=-0.5), [b_kss], [b_kss])
                    cx.op("dve", lambda e: e.tensor_scalar(out=kscale[:, 2 * blk:2 * blk + 2, :],
                                                           in0=kss[:].rearrange("p (a h) -> p a h", a=2),
                                                           scalar1=192.0 ** -0.5, scalar2=None, op0=ALU.mult),
                          [b_kss], [b_ksc])
                    stop_at(26)
                    for st2 in range(2):
                        for half in range(2):
                            g, bg = nextG()
                            cx.mm(g[:, 0:512],
                                  [(ckvn[:, kc, st2 * 128:(st2 + 1) * 128],
                                    wukv_v[:, kc, 1024 + half * 512:1024 + (half + 1) * 512]) for kc in range(2)],
                                  cur_w + [b_ckvn], [bg])
                            if half == 0:
                                cx.op("act", lambda e: e.copy(out=V[:, 2 * blk + st2, 0:512], in_=g[:, 0:512]), [bg], [b_V])
                            else:
                                cx.op("dve", lambda e: e.tensor_copy(out=V[:, 2 * blk + st2, 512:1024], in_=g[:, 0:512]),
                                      [bg], [b_V])
                    stop_at(27)
                    nkt = 2 * blk + 2

                    def qprep(h, qb):
                        g, bg = nextG()
                        cx.mm(g[:, 0:T], [(wuq_v[:, kc, h * 128:(h + 1) * 128], cqn[:, kc, :]) for kc in range(3)],
                              cur_w + [b_cqn], [bg])
                        g2, bg2 = nextG()
                        cx.mm(g2[0:64, 0:T], [(wuq_v[:, kc, 1024 + h * 64:1024 + (h + 1) * 64], cqn[:, kc, :]) for kc in range(3)],
                              cur_w + [b_cqn], [bg2])
                        cx.mm(g2[0:64, T:2 * T], [(wuq_v[:, kc, 1536 + h * 64:1536 + (h + 1) * 64], cqn[:, kc, :]) for kc in range(3)],
                              cur_w + [b_cqn], [bg2])
                        cx.op("act", lambda e: e.copy(out=qtmp[:], in_=g[:, 0:T]), [bg], [b_qtmp])
                        cx.op("act", lambda e: e.copy(out=ua[:], in_=g2[0:64, 0:T]), [bg2], [b_ua])
                        cx.op("act", lambda e: e.copy(out=ub[:], in_=g2[0:64, T:2 * T]), [bg2], [b_ub])
                        yield
                        cx.op("act", lambda e: e.activation(out=sqq[:], in_=qtmp[:], func=AF.Square), [b_qtmp], [b_sqq])
                        cx.op("act", lambda e: e.activation(out=sqqr[:], in_=ua[:], func=AF.Square), [b_ua], [b_sqqr])
                        yield
                        g3, bg3 = nextG()
                        cx.mm(g3[:, 0:T], [(onesb[:], sqq[:]), (onesb[0:64, :], sqqr[:])], [b_onesb, b_sqq, b_sqqr], [bg3])
                        rstd_from_ps(g3[:, 0:T], bg3, rq[:], b_rq, 1.0 / 192, EPS)
                        yield
                        cx.op("dve", lambda e: e.scalar_tensor_tensor(out=qn[qb][:], in0=qtmp[:], scalar=cvec[:, 0:1], in1=rq[:],
                                                                      op0=ALU.mult, op1=ALU.mult),
                              [b_qtmp, b_cvec, b_rq], [b_qn[qb]])
                        cx.op("dve", lambda e: e.scalar_tensor_tensor(out=tA[:], in0=ua[:], scalar=vec[0:64, 15:16],
                                                                      in1=cosT[:], op0=ALU.mult, op1=ALU.mult),
                              [b_ua, b_vec, b_cos], [b_tA])
                        yield
                        cx.op("dve", lambda e: e.scalar_tensor_tensor(out=tB[:], in0=ub[:], scalar=cvec[0:64, 1:2],
                                                                      in1=sinT[:], op0=ALU.mult, op1=ALU.mult),
                              [b_ub, b_cvec, b_sin], [b_tB])
                        cx.op("dve", lambda e: e.tensor_tensor(out=tA[:], in0=tA[:], in1=tB[:], op=ALU.add),
                              [b_tA, b_tB], [b_tA])
                        cx.op("dve", lambda e: e.tensor_tensor(out=qr[qb][:], in0=tA[:], in1=rq[0:64, :], op=ALU.mult),
                              [b_tA, b_rq], [b_qr[qb]])
                        yield

                    def attend(h, qb):
                        def s_mm(jt_):
                            gS_, bS_ = Sb[jt_ % 2]
                            cx.mm(gS_[:, 0:T], [(KT[:, h, jt_ * 128:(jt_ + 1) * 128], qn[qb][:]),
                                                (kr[0:64, jt_ * 128:(jt_ + 1) * 128], qr[qb][:])],
                                  [b_KT, b_kr, b_qn[qb], b_qr[qb]], [bS_])
                            return gS_, bS_
                        pend = s_mm(0)
                        for jt in range(nkt):
                            gS, bS = pend
                            if jt + 1 < nkt:
                                pend = s_mm(jt + 1)
                            pi = jt % 2
                            cx.op("act", lambda e: e.activation(out=Pt[pi][:], in_=gS[:, 0:T], func=AF.Exp,
                                                                scale=kscale[:, jt, h:h + 1]), [bS, b_ksc], [b_Pt[pi]])
                            if jt >= 2 * blk:
                                d_ = jt - 2 * blk
                                cx.op("dve", lambda e: e.tensor_tensor(out=Pt[pi][:], in0=Pt[pi][:],
                                                                       in1=amask[:, d_ * T:(d_ + 1) * T], op=ALU.mult),
                                      [b_Pt[pi], b_am], [b_Pt[pi]])
                            cx.mm(O_ps[:, 0:T], [(V[:, jt, h * 128:(h + 1) * 128], Pt[pi][:])], [b_V, b_Pt[pi]], [b_O],
                                  start=(jt == 0), stop=(jt == nkt - 1))
                            cx.mm(R_ps[:, 0:T], [(onesb[:], Pt[pi][:])], [b_onesb, b_Pt[pi]], [b_R],
                                  start=(jt == 0), stop=(jt == nkt - 1))
                            yield
                        cx.op("dve", lambda e: e.reciprocal(out=rinv[:], in_=R_ps[:, 0:T]), [b_R], [b_rinv])
                        cx.op("dve", lambda e: e.tensor_tensor(out=ob[:], in0=O_ps[:, 0:T], in1=rinv[:], op=ALU.mult),
                              [b_O, b_rinv], [b_ob])
                        cx.op("dve", lambda e: e.tensor_tensor(out=y[:, h, :], in0=ob[:], in1=szall[:, h, :], op=ALU.mult),
                              [b_ob, b_sz], [b_y])
                        yield

                    interleave(qprep(0, 0))
                    for h_ in range(8):
                        interleave(attend(h_, h_ % 2), qprep(h_ + 1, (h_ + 1) % 2) if h_ + 1 < 8 else None)
                    stop_at(29)
                    outproj_store(wo_v, 8, s, blk, par)
            cx.barrier()
            lst.close()

        try:
            for li, (kind, j) in enumerate(layers):
                src = xT if li == 0 else oT
                if kind == "a":
                    layer_a(j, src)
                else:
                    layer_c(j, src)
        except StopBuild:
            for l_ in open_lst:
                l_.close()
        cx.final_wait()
    return nc


def _consts():
    c = {}
    c["c_ones"] = np.ones((128, 128), np.float32)
    s_ = np.arange(64)
    c["c_tri"] = (s_[:, None] <= s_[None, :]).astype(np.float32)
    id4 = np.zeros((4, 8), np.float32)
    id4[:, 0:4] = np.eye(4)
    id4[:, 4:8] = np.eye(4) / 16.0
    c["c_id4"] = id4
    sel = np.zeros((4, 4, 128), np.float32)
    for h in range(4):
        sel[h, h, :] = 1.0
    c["c_sel"] = sel.reshape(4, 512)
    jj = np.arange(64)
    inv = (10000.0 ** (-(np.arange(0, 64, 2, dtype=np.float32)) / 64.0)).astype(np.float32)
    fr = np.zeros((64, 2), np.float32)
    fr[:, 0] = (inv.astype(np.float64)[jj % 32] / (2.0 * np.pi)).astype(np.float32)
    fr[:, 1] = np.where(jj < 32, -1.0, 1.0)
    c["c_freq"] = fr
    am = np.zeros((128, 2, T), np.float32)
    s2 = np.arange(128)[:, None] // 64
    tq = np.arange(T)[None, :] // 64
    for d_ in range(2):
        am[:, d_, :] = ((2 * d_ + s2) <= tq).astype(np.float32)
    c["c_amask"] = am.reshape(128, 2 * T)
    pf = np.ones((128, 64), np.float32)
    for g, w in enumerate((2, 4, 8, 16)):
        t = np.arange(16)
        pf[:, g * 16:(g + 1) * 16] = (w / np.minimum(t + 1, w)).astype(np.float32)[None, :]
    c["c_pfix"] = pf
    return c


def _pvec(v):
    v = np.asarray(v, np.float32)
    return np.ascontiguousarray(v.reshape(-1, 128).T)


def _prep_a(inp, j):
    d = {}
    d["a_win%d" % j] = np.ascontiguousarray(inp["a_w_in"][j], dtype=np.float32)
    d["a_wq%d" % j] = np.ascontiguousarray(inp["a_w_q"][j], dtype=np.float32)
    d["a_wk%d" % j] = np.ascontiguousarray(inp["a_w_k"][j], dtype=np.float32)
    d["a_wv%d" % j] = np.ascontiguousarray(inp["a_w_v"][j], dtype=np.float32)
    d["a_wpool%d" % j] = np.ascontiguousarray(inp["a_pool_w"][j], dtype=np.float32)
    d["a_wout%d" % j] = np.ascontiguousarray(inp["a_w_out"][j], dtype=np.float32)
    cw = inp["a_conv_w"][j]
    vec = np.concatenate([_pvec(inp["a_norm_g"][j])] + [_pvec(cw[k]) for k in range(4)] +
                         [_pvec(inp["a_conv_b"][j]), _pvec(inp["a_head_norm_g"][j]), _pvec(inp["a_skip"][j]),
                          _pvec(inp["a_pool_scale"][j])], axis=1)
    d["a_vec%d" % j] = np.ascontiguousarray(vec, dtype=np.float32)
    d["a_bif%d" % j] = np.ascontiguousarray(np.asarray(inp["a_b_if"][j], np.float32).reshape(2, 4).T)
    return d


def _prep_c(inp, j):
    d = {}
    perm = (np.arange(64) + 32) % 64
    win = np.asarray(inp["c_w_in"][j], np.float32)
    d["c_win%d" % j] = np.ascontiguousarray(np.concatenate([win, win[:, 640 + perm]], axis=1))
    wuq = np.asarray(inp["c_w_uq"][j], np.float32)
    hh_ = np.arange(8)[:, None]
    nope = (hh_ * 192 + np.arange(128)[None, :]).reshape(-1)
    rope = (hh_ * 192 + 128 + np.arange(64)[None, :]).reshape(-1)
    rot = (hh_ * 192 + 128 + perm[None, :]).reshape(-1)
    d["c_wuq%d" % j] = np.ascontiguousarray(wuq[:, np.concatenate([nope, rope, rot])])
    wukv = np.asarray(inp["c_w_ukv"][j], np.float32)
    kidx = (hh_ * 256 + np.arange(128)[None, :]).reshape(-1)
    vidx = (hh_ * 256 + 128 + np.arange(128)[None, :]).reshape(-1)
    d["c_wukv%d" % j] = np.ascontiguousarray(wukv[:, np.concatenate([kidx, vidx])])
    d["c_wout%d" % j] = np.ascontiguousarray(inp["c_w_out"][j], dtype=np.float32)
    qg = np.asarray(inp["c_qn_g"][j], np.float32)
    kg = np.asarray(inp["c_kn_g"][j], np.float32)

    def col64(v):
        o_ = np.zeros((128, 1), np.float32)
        o_[:64, 0] = v
        return o_
    vec = np.concatenate([_pvec(inp["c_norm_g"][j]), _pvec(inp["c_q_norm_g"][j]), _pvec(inp["c_kv_norm_g"][j]),
                          qg[:128, None], kg[:128, None], col64(qg[128:]), col64(qg[128:][perm]),
                          col64(kg[128:]), col64(kg[128:][perm])], axis=1)
    d["c_vec%d" % j] = np.ascontiguousarray(vec, dtype=np.float32)
    return d


LAYERS = [("a", 0), ("c", 0), ("a", 1), ("c", 1)]
_CACHE = {}


def run(inputs, layers, ns, n_cores):
    key = (tuple(layers), ns)
    if key not in _CACHE:
        _CACHE[key] = build_program(ns, layers)
    nc = _CACHE[key]
    x = np.asarray(inputs["x"], np.float32)
    posi = np.asarray(inputs["positions"], np.int32)
    shared = dict(_consts())
    for j in sorted({j for k, j in layers if k == "a"}):
        shared.update(_prep_a(inputs, j))
    for j in sorted({j for k, j in layers if k == "c"}):
        shared.update(_prep_c(inputs, j))
    in_maps = []
    for c in range(n_cores):
        m = dict(shared)
        m["xT"] = np.ascontiguousarray(x[c * ns:(c + 1) * ns].transpose(0, 2, 1))
        m["pos"] = np.ascontiguousarray(posi[c * ns:(c + 1) * ns])
        in_maps.append(m)
    res = run_bass_kernel_spmd(nc, in_maps, core_ids=list(range(n_cores)))
    outs = [np.asarray(r["oT"]).transpose(0, 2, 1) for r in res.results]
    return np.ascontiguousarray(np.concatenate(outs, axis=0), dtype=np.float32)


def kernel(**inputs):
    return run(inputs, LAYERS, 4, 8)
```

```python
import numpy as np
from contextlib import ExitStack
import concourse.bass as bass
import concourse.mybir as mybir
from concourse.bass_utils import run_bass_kernel_spmd

F32 = mybir.dt.float32
BF16 = mybir.dt.bfloat16
I32 = mybir.dt.int32
ALU = mybir.AluOpType
AF = mybir.ActivationFunctionType
AX = mybir.AxisListType

D = 1024
S = 2048
T = 256
NB = S // T
NCH = T // 64
A_IN = 5128
EPS = 1e-6
PI = float(np.pi)


class StopBuild(Exception):
    pass


import os
DEBUG_STOP = int(os.environ.get("KSTOP", "0"))


def stop_at(n):
    if DEBUG_STOP == n:
        raise StopBuild()


class Buf:
    __slots__ = ("name", "w", "r", "dsem", "dval", "excl")

    def __init__(self, name, excl=False):
        self.name = name
        self.excl = excl
        self.w = None
        self.r = {}
        self.dsem = None
        self.dval = 0


class Ctx:
    def __init__(self, nc, stack):
        self.nc = nc
        self.stack = stack
        self.E = {"pe": nc.tensor, "act": nc.scalar, "dve": nc.vector, "pool": nc.gpsimd, "sp": nc.sync}
        self.sem = {e: stack.enter_context(nc.semaphore("sem_" + e)) for e in ("pe", "act", "dve", "pool")}
        self.semid = {id(v): k for k, v in self.sem.items()}
        self.cnt = {e: 0 for e in self.sem}
        self.seen = {e: {} for e in self.E}
        self.bufs = []
        self.nd = 0

    def buf(self, name, excl=False):
        b = Buf(name, excl)
        self.bufs.append(b)
        return b

    def _wait(self, eng, ev, raw):
        sem, val = ev
        own = self.sem.get(eng)
        if own is sem:
            if eng == "pe" or not raw:
                return
        k = id(sem)
        if self.seen[eng].get(k, 0) >= val:
            return
        self.E[eng].wait_ge(sem, val)
        self.seen[eng][k] = val

    def _deps(self, eng, reads, writes):
        for b in reads:
            if b.w is not None:
                self._wait(eng, b.w, True)
            if b.excl:
                for ev in b.r.values():
                    self._wait(eng, ev, False)
        for b in writes:
            if b.w is not None:
                self._wait(eng, b.w, False)
            for ev in b.r.values():
                self._wait(eng, ev, False)

    def _commit(self, ev, reads, writes):
        k = id(ev[0])
        for b in reads:
            b.r[k] = ev
        for b in writes:
            b.w = ev
            b.r = {}

    def op(self, eng, fn, reads=(), writes=()):
        self._deps(eng, reads, writes)
        ins = fn(self.E[eng])
        self.cnt[eng] += 1
        ins.then_inc(self.sem[eng], 1)
        self._commit((self.sem[eng], self.cnt[eng]), reads, writes)

    def mm(self, out, pairs, reads, writes, start=True, stop=True):
        self._deps("pe", reads, writes)
        n = len(pairs)
        ins = None
        for i, (l, r) in enumerate(pairs):
            ins = self.nc.tensor.matmul(out, lhsT=l, rhs=r, start=(start and i == 0), stop=(stop and i == n - 1))
        self.cnt["pe"] += 1
        ins.then_inc(self.sem["pe"], 1)
        self._commit((self.sem["pe"], self.cnt["pe"]), reads, writes)

    def dma(self, out, in_, reads, writes, sbuf):
        self._deps("sp", reads, writes)
        if sbuf.dsem is None:
            self.nd += 1
            sbuf.dsem = self.stack.enter_context(self.nc.semaphore("dsem%d" % self.nd))
        ins = self.nc.sync.dma_start(out=out, in_=in_)
        sbuf.dval += 16
        ins.then_inc(sbuf.dsem, 16)
        self._commit((sbuf.dsem, sbuf.dval), reads, writes)

    def barrier(self):
        for e in ("act", "dve", "pool", "sp", "pe"):
            for e2 in self.sem:
                if self.cnt[e2] > 0:
                    self._wait(e, (self.sem[e2], self.cnt[e2]), True)
            for b in self.bufs:
                if b.dsem is not None and b.dval > 0:
                    self._wait(e, (b.dsem, b.dval), True)

    def final_wait(self):
        for b in self.bufs:
            if b.dsem is not None:
                self._wait("sp", (b.dsem, b.dval), True)
        for e in self.sem:
            self._wait("sp", (self.sem[e], self.cnt[e]), True)


def interleave(*gens):
    gens = [g for g in gens if g is not None]
    while gens:
        for g in list(gens):
            try:
                next(g)
            except StopIteration:
                gens.remove(g)


def build_program(ns, layers):
    nc = bass.Bass("TRN2", target_bir_lowering=False)

    def din(name, shape, dt=F32):
        return nc.dram_tensor(name, list(shape), dt, kind="ExternalInput").ap()

    xT = din("xT", [ns, D, S])
    pos = din("pos", [ns, S], I32)
    oT = nc.dram_tensor("oT", [ns, D, S], F32, kind="ExternalOutput").ap()
    W = {}
    for j in sorted({j for k, j in layers if k == "a"}):
        W["a", j] = dict(
            win=din("a_win%d" % j, [D, A_IN]), wq=din("a_wq%d" % j, [4, 256, 256]),
            wk=din("a_wk%d" % j, [4, 256, 256]), wv=din("a_wv%d" % j, [4, 256, 256]),
            wpool=din("a_wpool%d" % j, [4, 256, 256]), wout=din("a_wout%d" % j, [2048, D]),
            vec=din("a_vec%d" % j, [128, 72]), bif=din("a_bif%d" % j, [4, 2]))
    for j in sorted({j for k, j in layers if k == "c"}):
        W["c", j] = dict(
            win=din("c_win%d" % j, [D, 1792]), wuq=din("c_wuq%d" % j, [384, 2048]),
            wukv=din("c_wukv%d" % j, [256, 2048]), wout=din("c_wout%d" % j, [D, D]),
            vec=din("c_vec%d" % j, [128, 19]))
    c_ones = din("c_ones", [128, 128])
    c_tri = din("c_tri", [64, 64])
    c_id4 = din("c_id4", [4, 8])
    c_sel = din("c_sel", [4, 512])
    c_freq = din("c_freq", [64, 2])
    c_amask = din("c_amask", [128, 2 * T])
    c_pfix = din("c_pfix", [128, 64])

    with ExitStack() as st:
        cx = Ctx(nc, st)

        def sb(name, shape, dt=F32):
            return st.enter_context(nc.sbuf_tensor(name, list(shape), dt))

        def ps(name, shape, dt=F32):
            return st.enter_context(nc.psum_tensor(name, list(shape), dt))

        onesb = sb("onesb", [128, 128], BF16); b_onesb = cx.buf("onesb")
        tri = sb("tri", [64, 64]); b_tri = cx.buf("tri")
        id4 = sb("id4", [4, 8]); b_id4 = cx.buf("id4")
        sel = sb("sel", [4, 512]); b_sel = cx.buf("sel")
        freq = sb("freq", [64, 2]); b_freq = cx.buf("freq")
        amask = sb("amask", [128, 2 * T], BF16); b_am = cx.buf("am")
        pfix = sb("pfix", [128, 64]); b_pfix = cx.buf("pfix")
        for (t_, b_, src) in ((tri, b_tri, c_tri), (id4, b_id4, c_id4),
                              (sel, b_sel, c_sel), (freq, b_freq, c_freq), (pfix, b_pfix, c_pfix)):
            cx.dma(t_[:], src[:, :], [], [b_], b_)

        WCOLS = 67600
        arena = sb("arena", [128, WCOLS], BF16)
        cur_w = []
        stg = [sb("stg%d" % i, [128, 512]) for i in range(2)]
        b_stg = [cx.buf("stg%d" % i) for i in range(2)]
        cx.dma(stg[0][:, 0:128], c_ones[:, :], [], [b_stg[0]], b_stg[0])
        cx.op("dve", lambda e: e.tensor_copy(out=onesb[:], in_=stg[0][:, 0:128]), [b_stg[0]], [b_onesb])
        cx.dma(stg[1][:, 0:2 * T], c_amask[:, :], [], [b_stg[1]], b_stg[1])
        cx.op("dve", lambda e: e.tensor_copy(out=amask[:], in_=stg[1][:, 0:2 * T]), [b_stg[1]], [b_am])
        vec = sb("vec", [128, 72]); b_vec = cx.buf("vec")
        x32 = [sb("x32_0", [128, 8, T])]
        b_x32 = [cx.buf("x32_0")]
        ost = [sb("ost%d" % i, [128, T]) for i in range(2)]
        b_ost = [cx.buf("ost%d" % i) for i in range(2)]
        xn = sb("xn", [128, 8, T], BF16); b_xn = cx.buf("xn")
        rst = sb("rst", [128, T]); b_rst = cx.buf("rst")
        y = sb("y", [128, 16, T], BF16); b_y = cx.buf("y")
        sq = y[:, 0:8, :]; b_sq = b_y

        G = [ps("G%d" % i, [128, 512]) for i in range(4)]
        b_G = [cx.buf("G%d" % i, True) for i in range(4)]
        P4 = [ps("P%d" % i, [128, 512]) for i in range(4)]
        b_P4 = [cx.buf("P%d" % i, True) for i in range(4)]
        gi_ = [0]

        def nextG():
            i = gi_[0] % 4
            gi_[0] += 1
            return G[i], b_G[i]

        rr = [0]

        def rr_eng(opts=("act", "dve", "pool")):
            rr[0] += 1
            return opts[rr[0] % len(opts)]

        stg_i = [0]

        def load_w(dst_ap, src_ap, ncols, scale_ap=None, b_scale=None):
            c0 = 0
            while c0 < ncols:
                n = min(512, ncols - c0)
                i = stg_i[0] % 2
                stg_i[0] += 1
                cx.dma(stg[i][:, 0:n], src_ap[:, c0:c0 + n], [], [b_stg[i]], b_stg[i])
                eng = rr_eng()
                d = dst_ap[:, c0:c0 + n]
                s_ = stg[i][:, 0:n]
                bw = cx.buf("w")
                cur_w.append(bw)
                if scale_ap is None:
                    if eng == "act":
                        cx.op("act", lambda e: e.copy(out=d, in_=s_), [b_stg[i]], [bw])
                    else:
                        cx.op(eng, lambda e: e.tensor_copy(out=d, in_=s_), [b_stg[i]], [bw])
                else:
                    if eng == "act":
                        cx.op("act", lambda e: e.activation(out=d, in_=s_, func=AF.Copy, scale=scale_ap),
                              [b_stg[i], b_scale], [bw])
                    else:
                        cx.op(eng, lambda e: e.tensor_scalar(out=d, in0=s_, scalar1=scale_ap, scalar2=None,
                                                             op0=ALU.mult), [b_stg[i], b_scale], [bw])
                c0 += n

        def rstd_from_ps(ps_ap, b_ps, out_ap, b_out, inv_n, eps):
            cx.op("dve", lambda e: e.tensor_scalar(out=out_ap, in0=ps_ap, scalar1=inv_n, scalar2=eps,
                                                   op0=ALU.mult, op1=ALU.add), [b_ps], [b_out])
            cx.op("act", lambda e: e.activation(out=out_ap, in_=out_ap, func=AF.Ln), [b_out], [b_out])
            cx.op("act", lambda e: e.activation(out=out_ap, in_=out_ap, func=AF.Exp, scale=-0.5), [b_out], [b_out])

        def load_block(src, s, blk, par):
            t0 = blk * T
            src_ap = src[s].rearrange("(kc p) t -> p kc t", p=128)[:, :, t0:t0 + T]
            cx.dma(x32[par][:], src_ap, [b_dram[s][blk]], [b_x32[par]], b_x32[par])

        def norm_block(par):
            cx.op("act", lambda e: e.activation(out=sq[:], in_=x32[par][:], func=AF.Square), [b_x32[par]], [b_sq])
            g, bg = nextG()
            cx.mm(g[:, 0:T], [(onesb[:], sq[:, kc, :]) for kc in range(8)], [b_onesb, b_sq], [bg])
            rstd_from_ps(g[:, 0:T], bg, rst[:], b_rst, 1.0 / D, EPS)
            for kc in range(8):
                cx.op("dve", lambda e: e.tensor_tensor(out=xn[:, kc, :], in0=x32[par][:, kc, :],
                                                                          in1=rst[:], op=ALU.mult),
                      [b_x32[par], b_rst], [b_xn])

        def outproj_store(wout_v, nk, s, blk, par):
            for op_ in range(4):
                g, bg = nextG()
                for o2 in range(2):
                    oc = op_ * 2 + o2
                    cx.mm(g[:, o2 * T:(o2 + 1) * T],
                          [(wout_v[:, kc, oc * 128:(oc + 1) * 128], y[:, kc, :]) for kc in range(nk)],
                          cur_w + [b_y], [bg])
                for o2 in range(2):
                    oc = op_ * 2 + o2
                    oi = oc % 2
                    cx.op("dve", lambda e: e.tensor_tensor(out=ost[oi][:], in0=g[:, o2 * T:(o2 + 1) * T],
                                                           in1=x32[par][:, oc, :], op=ALU.add),
                          [bg, b_x32[par]], [b_ost[oi]])
                    cx.dma(oT[s][oc * 128:(oc + 1) * 128, blk * T:(blk + 1) * T], ost[oi][:],
                           [b_ost[oi]], [b_dram[s][blk]], b_ost[oi])

        open_lst = []
        b_dram = [[cx.buf("dram%d_%d" % (s, b)) for b in range(NB)] for s in range(ns)]

        def layer_a(j, src):
            w = W["a", j]

            lst = ExitStack()
            open_lst.append(lst)

            def sb(name, shape, dt=F32):
                return lst.enter_context(nc.sbuf_tensor(name + "_a%d" % j, list(shape), dt))

            bif = sb("bif", [4, 2]); b_bif = cx.buf("bif")
            nbif = sb("nbif", [4, 2]); b_nbif = cx.buf("nbif")
            xm = sb("xm", [128, 8, 16 + T], BF16); b_xm = [cx.buf("xm%d" % h) for h in range(4)]
            acc = sb("acc", [128, 2, T]); b_acc = cx.buf("acc")
            flB = acc[:, 0, :]; b_flB = b_acc
            xc = sb("xc", [128, 2, T], BF16); b_xc = cx.buf("xc")
            tom = sb("tom", [128, 2, T], BF16); b_tom = cx.buf("tom")
            szm = sb("szm", [128, 2, T], BF16); b_szm = cx.buf("szm")
            qh = sb("qh", [128, 2, T], BF16); b_qh = cx.buf("qh")
            kh = sb("kh", [128, 2, T], BF16); b_kh = cx.buf("kh")
            vsb = sb("vsb", [64, NCH, 256], BF16); b_vsb = cx.buf("vsb")
            kwsb = sb("kwsb", [64, NCH, 256], BF16); b_kwsb = cx.buf("kwsb")
            sTw = [sb("sTw%d" % i, [64, 64], BF16) for i in range(NCH)]; b_sTw = [cx.buf("sTw%d" % i) for i in range(NCH)]

            Cf = sb("Cf", [128, 4, 2, 256]); b_Cf = [cx.buf("Cf%d" % h) for h in range(4)]
            nf = sb("nf", [128, 4, 2]); b_nf = [cx.buf("nf%d" % h) for h in range(4)]
            Cb = [sb("Cb%d" % i, [128, 2, 256], BF16) for i in range(2)]; b_Cb = [cx.buf("Cb%d" % i) for i in range(2)]
            nsc = sb("nsc", [128, 2]); b_nsc = cx.buf("nsc")
            nBc = [sb("nBc%d" % i, [128, 2, 128], BF16) for i in range(2)]; b_nBc = [cx.buf("nBc%d" % i) for i in range(2)]
            nlf = sb("nlf", [4, T]); b_nlf = cx.buf("nlf")
            nlfT = sb("nlfT", [64, NCH * 4]); b_nlfT = cx.buf("nlfT")
            nb_sb = sb("nb_sb", [4, T]); b_nb = cx.buf("nb")
            a_sb = sb("a_sb", [4, T]); b_a = cx.buf("a")
            gi_sb = a_sb; b_gi = b_a
            Amax = sb("Amax", [4, NCH]); b_Amax = cx.buf("Amax")
            Mc = sb("Mc", [4, NCH]); b_Mc = cx.buf("Mc")
            mprev = sb("mprev", [4, NCH + 1]); b_mprev = cx.buf("mprev")
            w_sb = a_sb; b_w = b_a
            fl_sb = nb_sb; b_fl = b_nb
            wc_sb = sb("wc_sb", [4, NCH]); b_wc = cx.buf("wc")
            wsc = sb("wsc", [64, NCH * 4]); b_wsc = cx.buf("wsc")
            wcB = sb("wcB", [128, 4 * NCH]); b_wcB = cx.buf("wcB")
            den = sb("den", [128, T]); b_den = cx.buf("den")
            hh = sb("hh", [128, 2, T]); b_hh = cx.buf("hh")
            sqh = sb("sqh", [128, 2, T], BF16); b_sqh = cx.buf("sqh")
            rsth = sb("rsth", [128, T]); b_rsth = cx.buf("rsth")
            t2 = den; b_t2 = b_den
            xp = sb("xp", [128, 8, 16 + T], BF16); b_xp = [cx.buf("xp%d" % g) for g in range(4)]
            pA = sb("pA", [128, 2, 16 + T], BF16); b_pA = cx.buf("pA")
            pB = sb("pB", [128, 2, 16 + T], BF16); b_pB = cx.buf("pB")
            mixb = sb("mixb", [128, 2, T], BF16); b_mix = cx.buf("mix")
            szp = sb("szp", [128, 2, T], BF16); b_szp = cx.buf("szp")
            sT_ps, b_sTps = P4[0], b_P4[0]
            NT_ps, b_NT = P4[1], b_P4[1]
            Dn_ps, b_Dn = P4[2], b_P4[2]
            sTb = [(P4[0], b_P4[0]), (P4[3], b_P4[3])]
            cx.barrier()
            del cur_w[:]
            cx.dma(vec[:, 0:72], w["vec"][:, :], [], [b_vec], b_vec)
            cx.dma(bif[:], w["bif"][:, :], [], [b_bif], b_bif)
            cx.op("dve", lambda e: e.tensor_scalar(out=nbif[:], in0=bif[:], scalar1=-1.0, scalar2=None, op0=ALU.mult),
                  [b_bif], [b_nbif])
            o = 0
            win_v = arena[:, o:o + 8 * A_IN].rearrange("p (k n) -> p k n", k=8); o += 8 * A_IN
            wq_v = arena[:, o:o + 2048].rearrange("p (h c n) -> p h c n", h=4, c=2); o += 2048
            wk_v = arena[:, o:o + 2048].rearrange("p (h c n) -> p h c n", h=4, c=2); o += 2048
            wv_v = arena[:, o:o + 2048].rearrange("p (h c n) -> p h c n", h=4, c=2); o += 2048
            wp_v = arena[:, o:o + 2048].rearrange("p (h c n) -> p h c n", h=4, c=2); o += 2048
            wo_v = arena[:, o:o + 16 * D].rearrange("p (k n) -> p k n", k=16); o += 16 * D
            assert o <= WCOLS
            win_src = w["win"].rearrange("(k p) n -> p k n", p=128)
            for kc in range(8):
                load_w(win_v[:, kc, :], win_src[:, kc, :], A_IN, vec[:, kc:kc + 1], b_vec)
            for (dv, nm) in ((wq_v, "wq"), (wk_v, "wk"), (wv_v, "wv"), (wp_v, "wpool")):
                srcw = w[nm].rearrange("h (c p) n -> p h c n", p=128)
                for h in range(4):
                    for c in range(2):
                        load_w(dv[:, h, c, :], srcw[:, h, c, :], 256)
            wo_src = w["wout"].rearrange("(k p) n -> p k n", p=128)
            for kc in range(16):
                load_w(wo_v[:, kc, :], wo_src[:, kc, :], D)

            V_CW, V_CB, V_HNG, V_SKIP, V_PS = 8, 40, 48, 56, 64
            stop_at(1)

            def inproj2(col0):
                g, bg = nextG()
                for oc in range(2):
                    c0 = col0 + oc * 128
                    cx.mm(g[:, oc * T:(oc + 1) * T],
                          [(win_v[:, kc, c0:c0 + 128], xn[:, kc, :]) for kc in range(8)], cur_w + [b_xn], [bg])
                return g[:, 0:2 * T].rearrange("p (a t) -> p a t", a=2), bg

            for s in range(ns):
                cx.op("pool", lambda e: e.memset(Cf[:], 0.0), [], b_Cf)
                cx.op("pool", lambda e: e.memset(nf[:], 0.0), [], b_nf)
                cx.op("pool", lambda e: e.memset(xm[:], 0.0), [], b_xm)
                cx.op("pool", lambda e: e.memset(xp[:], 0.0), [], b_xp)
                cx.op("pool", lambda e: e.memset(mprev[:], 0.0), [], [b_mprev])
                for blk in range(NB):
                    par = 0
                    load_block(src, s, blk, par)
                    norm_block(par)
                    stop_at(2)
                    g, bg = nextG()
                    cx.mm(g[0:4, 0:T], [(win_v[:, kc, 2048:2052], xn[:, kc, :]) for kc in range(8)], cur_w + [b_xn], [bg])
                    cx.mm(g[0:4, T:2 * T], [(win_v[:, kc, 2052:2056], xn[:, kc, :]) for kc in range(8)], cur_w + [b_xn], [bg])
                    cx.op("act", lambda e: e.activation(out=gi_sb[:], in_=g[0:4, 0:T], func=AF.Identity, bias=bif[:, 0:1]),
                          [bg, b_bif], [b_gi])
                    cx.op("act", lambda e: e.activation(out=nlf[:], in_=g[0:4, T:2 * T], func=AF.Exp, scale=-1.0,
                                                        bias=nbif[:, 1:2]), [bg, b_nbif], [b_nlf])
                    cx.op("act", lambda e: e.activation(out=nlf[:], in_=nlf[:], func=AF.Ln, bias=1.0), [b_nlf], [b_nlf])
                    g2, bg2 = nextG()
                    for c in range(NCH):
                        cx.mm(g2[0:64, c * 4:(c + 1) * 4], [(nlf[0:4, c * 64:(c + 1) * 64], id4[0:4, 0:4])],
                              [b_nlf, b_id4], [bg2])
                    cx.op("act", lambda e: e.copy(out=nlfT[:], in_=g2[0:64, 0:NCH * 4]), [bg2], [b_nlfT])
                    g3, bg3 = nextG()
                    for c in range(NCH):
                        cx.mm(g3[0:4, c * 64:(c + 1) * 64], [(nlfT[:, c * 4:(c + 1) * 4], tri[:])], [b_nlfT, b_tri], [bg3])
                    cx.op("act", lambda e: e.copy(out=nb_sb[:], in_=g3[0:4, 0:T]), [bg3], [b_nb])
                    cx.op("dve", lambda e: e.tensor_tensor(out=a_sb[:], in0=a_sb[:], in1=nb_sb[:], op=ALU.add),
                          [b_a, b_nb], [b_a])
                    cx.op("dve", lambda e: e.tensor_reduce(out=Amax[:], in_=a_sb[:].rearrange("p (c t) -> p c t", c=NCH),
                                                           axis=AX.X, op=ALU.max), [b_a], [b_Amax])
                    cx.op("dve", lambda e: e.tensor_copy(out=mprev[:, 0:1], in_=mprev[:, NCH:NCH + 1]), [b_mprev], [b_mprev])
                    for c in range(NCH):
                        cx.op("dve", lambda e: e.tensor_tensor(out=Mc[:, c:c + 1], in0=mprev[:, c:c + 1],
                                                               in1=Amax[:, c:c + 1], op=ALU.max),
                              [b_mprev, b_Amax], [b_Mc])
                        cx.op("dve", lambda e: e.tensor_tensor(out=mprev[:, c + 1:c + 2], in0=Mc[:, c:c + 1],
                                                               in1=nb_sb[:, c * 64 + 63:c * 64 + 64], op=ALU.subtract),
                              [b_Mc, b_nb], [b_mprev])
                    for c in range(NCH):
                        cs = slice(c * 64, (c + 1) * 64)
                        cx.op("dve", lambda e: e.tensor_scalar(out=a_sb[:, cs], in0=a_sb[:, cs], scalar1=Mc[:, c:c + 1],
                                                               scalar2=None, op0=ALU.subtract), [b_a, b_Mc], [b_a])
                        cx.op("dve", lambda e: e.tensor_scalar(out=nb_sb[:, cs], in0=nb_sb[:, cs], scalar1=Mc[:, c:c + 1],
                                                               scalar2=None, op0=ALU.subtract), [b_nb, b_Mc], [b_nb])
                    cx.op("dve", lambda e: e.tensor_tensor(out=wc_sb[:], in0=mprev[:, 0:NCH], in1=Mc[:], op=ALU.subtract),
                          [b_mprev, b_Mc], [b_wc])
                    cx.op("act", lambda e: e.activation(out=w_sb[:], in_=a_sb[:], func=AF.Exp), [b_a], [b_w])
                    cx.op("act", lambda e: e.activation(out=fl_sb[:], in_=nb_sb[:], func=AF.Exp), [b_nb], [b_fl])
                    cx.op("act", lambda e: e.activation(out=wc_sb[:], in_=wc_sb[:], func=AF.Exp), [b_wc], [b_wc])
                    g4, bg4 = nextG()
                    for c in range(NCH):
                        cx.mm(g4[0:64, c * 4:(c + 1) * 4], [(w_sb[0:4, c * 64:(c + 1) * 64], id4[0:4, 4:8])],
                              [b_w, b_id4], [bg4])
                    cx.op("act", lambda e: e.copy(out=wsc[:], in_=g4[0:64, 0:NCH * 4]), [bg4], [b_wsc])
                    g5, bg5 = nextG()
                    for h in range(4):
                        cx.mm(g5[:, h * NCH:(h + 1) * NCH], [(sel[0:4, h * 128:(h + 1) * 128], wc_sb[:])], [b_sel, b_wc], [bg5])
                    cx.op("dve", lambda e: e.tensor_copy(out=wcB[:], in_=g5[:, 0:4 * NCH]), [bg5], [b_wcB])
                    stop_at(3)
                    def head_gen(h):
                        pv, bp = inproj2(h * 256)
                        xmh = xm[:, 2 * h:2 * h + 2, :]
                        cx.op("act", lambda e: e.copy(out=xmh[:, :, 16:16 + T], in_=pv), [bp], [b_xm[h]])
                        for oc in range(2):
                            ch = 2 * h + oc
                            for k in range(4):
                                src_k = xm[:, ch, 13 + k:13 + k + T]
                                cwk = vec[:, V_CW + k * 8 + ch:V_CW + k * 8 + ch + 1]
                                if k == 0:
                                    cx.op("dve", lambda e: e.tensor_scalar(out=acc[:, oc, :], in0=src_k, scalar1=cwk,
                                                                            scalar2=None, op0=ALU.mult),
                                          [b_xm[h], b_vec], [b_acc])
                                else:
                                    cx.op("dve", lambda e: e.scalar_tensor_tensor(out=acc[:, oc, :], in0=src_k, scalar=cwk,
                                                                                   in1=acc[:, oc, :], op0=ALU.mult,
                                                                                   op1=ALU.add),
                                          [b_xm[h], b_vec, b_acc], [b_acc])
                            cx.op("act", lambda e: e.activation(out=xc[:, oc, :], in_=acc[:, oc, :], func=AF.Silu,
                                                                bias=vec[:, V_CB + ch:V_CB + ch + 1]),
                                  [b_acc, b_vec], [b_xc])
                        cx.op("pool", lambda e: e.tensor_copy(out=xmh[:, :, 0:16], in_=xmh[:, :, T:T + 16]),
                              [b_xm[h]], [b_xm[h]])
                        stop_at(7)
                        yield
                        pv, bp = inproj2(1024 + h * 256)
                        cx.op("act", lambda e: e.activation(out=tom[:], in_=pv, func=AF.Tanh, scale=0.5), [bp], [b_tom])
                        pv, bp = inproj2(2056 + h * 256)
                        cx.op("act", lambda e: e.activation(out=szm[:], in_=pv, func=AF.Silu), [bp], [b_szm])
                        stop_at(8)
                        yield
                        for (wv_, dst, bd, eng) in ((wq_v, qh, b_qh, "dve"), (wk_v, kh, b_kh, "act")):
                            g, bg = nextG()
                            for oc in range(2):
                                cx.mm(g[:, oc * T:(oc + 1) * T],
                                      [(wv_[:, h, cc, oc * 128:(oc + 1) * 128], xc[:, cc, :]) for cc in range(2)],
                                      cur_w + [b_xc], [bg])
                            pvv = g[:, 0:2 * T].rearrange("p (a t) -> p a t", a=2)
                            if eng == "dve":
                                cx.op("dve", lambda e: e.tensor_copy(out=dst[:], in_=pvv), [bg], [bd])
                            else:
                                cx.op("act", lambda e: e.copy(out=dst[:], in_=pvv), [bg], [bd])
                        stop_at(9)
                        yield
                        for c in range(NCH):
                            cs = slice(c * 64, (c + 1) * 64)
                            g, bg = nextG()
                            cx.mm(g[0:64, 0:256], [(xm[:, 2 * h + cc, 16 + c * 64:16 + (c + 1) * 64], wv_v[:, h, cc, :]) for cc in range(2)],
                                  cur_w + [b_xm[h]], [bg])
                            stop_at(14)
                            cx.mm(g[0:64, 256:512], [(xc[:, cc, cs], wk_v[:, h, cc, :]) for cc in range(2)],
                                  cur_w + [b_xc], [bg])
                            stop_at(15)
                            cx.op("act", lambda e: e.copy(out=vsb[:, c, 0:256], in_=g[0:64, 0:256]), [bg], [b_vsb])
                            stop_at(16)
                            cx.op("act", lambda e: e.activation(out=kwsb[:, c, :], in_=g[0:64, 256:512], func=AF.Copy,
                                                                scale=wsc[:, c * 4 + h:c * 4 + h + 1]), [bg, b_wsc], [b_kwsb])
                        stop_at(10)
                        yield
                        Ug = []
                        for c in range(NCH):
                            cs = slice(c * 64, (c + 1) * 64)
                            sTp, b_sTp = sTb[c % 2]
                            s0 = (c // 2) * 64
                            cx.mm(sTp[0:64, s0:s0 + 64], [(kh[:, dc, cs], qh[:, dc, cs]) for dc in range(2)],
                                  [b_kh, b_qh], [b_sTp])
                            gU, bU = nextG()
                            for dc in range(2):
                                cx.mm(gU[:, dc * 256:(dc + 1) * 256],
                                      [(kwsb[:, c, dc * 128:(dc + 1) * 128], vsb[:, c, 0:256])], [b_kwsb, b_vsb], [bU])
                                cx.mm(sTp[:, 256 + 2 * c + dc:256 + 2 * c + dc + 1],
                                      [(kwsb[:, c, dc * 128:(dc + 1) * 128], onesb[0:64, 0:1])], [b_kwsb, b_onesb], [b_sTp])
                            cx.op("dve", lambda e: e.scalar_tensor_tensor(out=sTw[c][:], in0=sTp[0:64, s0:s0 + 64],
                                                                          scalar=wsc[:, c * 4 + h:c * 4 + h + 1], in1=tri[:],
                                                                          op0=ALU.mult, op1=ALU.mult),
                                  [b_sTp, b_wsc, b_tri], [b_sTw[c]])
                            Ug.append((gU, bU))
                        for c in range(NCH):
                            cs = slice(c * 64, (c + 1) * 64)
                            ci = c % 2
                            wcol = wcB[:, h * NCH + c:h * NCH + c + 1]
                            cx.op("act", lambda e: e.activation(out=Cb[ci][:], in_=Cf[:, h], func=AF.Copy, scale=wcol),
                                  [b_Cf[h], b_wcB], [b_Cb[ci]])
                            cx.op("dve", lambda e: e.tensor_scalar(out=nsc[:], in0=nf[:, h, :], scalar1=wcol, scalar2=None,
                                                                   op0=ALU.mult), [b_nf[h], b_wcB], [b_nsc])
                            for dc in range(2):
                                cx.op("dve", lambda e: e.tensor_scalar(out=nBc[ci][:, dc, :], in0=onesb[:],
                                                                       scalar1=nsc[:, dc:dc + 1], scalar2=None,
                                                                       op0=ALU.mult), [b_onesb, b_nsc], [b_nBc[ci]])
                            gU, bU = Ug[c]
                            cx.op("dve", lambda e: e.scalar_tensor_tensor(
                                out=Cf[:, h], in0=Cf[:, h], scalar=wcol,
                                in1=gU[:, 0:512].rearrange("p (a n) -> p a n", a=2), op0=ALU.mult, op1=ALU.add),
                                [b_Cf[h], b_wcB, bU], [b_Cf[h]])
                            cx.op("dve", lambda e: e.scalar_tensor_tensor(
                                out=nf[:, h, :], in0=nf[:, h, :], scalar=wcol, in1=sTb[c % 2][0][:, 256 + 2 * c:256 + 2 * c + 2],
                                op0=ALU.mult, op1=ALU.add), [b_nf[h], b_wcB, sTb[c % 2][1]], [b_nf[h]])
                            for jv in range(2):
                                cx.mm(NT_ps[:, jv * T + c * 64:jv * T + (c + 1) * 64],
                                      [(vsb[:, c, jv * 128:(jv + 1) * 128], sTw[c][:])] +
                                      [(Cb[ci][:, dc, jv * 128:(jv + 1) * 128], qh[:, dc, cs]) for dc in range(2)],
                                      [b_vsb, b_sTw[c], b_Cb[ci], b_qh], [b_NT])
                            cx.mm(Dn_ps[:, cs], [(onesb[0:64, :], sTw[c][:])] +
                                  [(nBc[ci][:, dc, :], qh[:, dc, cs]) for dc in range(2)],
                                  [b_onesb, b_sTw[c], b_nBc[ci], b_qh], [b_Dn])
                        stop_at(12)
                        yield
                        g5, bg5 = nextG()
                        cx.mm(g5[:, 0:T], [(sel[0:4, h * 128:(h + 1) * 128], fl_sb[:])], [b_sel, b_fl], [bg5])
                        cx.op("act", lambda e: e.copy(out=flB, in_=g5[:, 0:T]), [bg5], [b_flB])
                        cx.op("act", lambda e: e.activation(out=den[:], in_=Dn_ps[:, 0:T], func=AF.Abs), [b_Dn], [b_den])
                        cx.op("dve", lambda e: e.tensor_tensor(out=den[:], in0=den[:], in1=flB,
                                                               op=ALU.max), [b_den, b_flB], [b_den])
                        cx.op("act", lambda e: e.activation(out=den[:], in_=den[:], func=AF.Ln), [b_den], [b_den])
                        cx.op("act", lambda e: e.activation(out=den[:], in_=den[:], func=AF.Exp, scale=-1.0), [b_den], [b_den])
                        for jv in range(2):
                            cx.op("dve", lambda e: e.tensor_tensor(out=hh[:, jv, :], in0=NT_ps[:, jv * T:(jv + 1) * T],
                                                                   in1=den[:], op=ALU.mult), [b_NT, b_den], [b_hh])
                        cx.op("dve", lambda e: e.scalar_tensor_tensor(out=hh[:], in0=tom[:], scalar=1.0, in1=hh[:],
                                                                       op0=ALU.add, op1=ALU.mult), [b_tom, b_hh], [b_hh])
                        cx.op("act", lambda e: e.activation(out=sqh[:], in_=hh[:], func=AF.Square), [b_hh], [b_sqh])
                        g, bg = nextG()
                        cx.mm(g[:, 0:T], [(onesb[:], sqh[:, jv, :]) for jv in range(2)], [b_onesb, b_sqh], [bg])
                        rstd_from_ps(g[:, 0:T], bg, rsth[:], b_rsth, 1.0 / 256, 4.0 * EPS)
                        yield
                        for jv in range(2):
                            ch = 2 * h + jv
                            cx.op("dve", lambda e: e.scalar_tensor_tensor(out=t2[:], in0=hh[:, jv, :],
                                                                          scalar=vec[:, V_HNG + ch:V_HNG + ch + 1],
                                                                          in1=rsth[:], op0=ALU.mult, op1=ALU.mult),
                                  [b_hh, b_vec, b_rsth], [b_t2])
                            cx.op("dve", lambda e: e.scalar_tensor_tensor(out=t2[:], in0=xc[:, jv, :],
                                                                           scalar=vec[:, V_SKIP + ch:V_SKIP + ch + 1],
                                                                           in1=t2[:], op0=ALU.mult, op1=ALU.add),
                                  [b_xc, b_vec, b_t2], [b_t2])
                            cx.op("dve", lambda e: e.tensor_tensor(out=y[:, ch, :], in0=t2[:], in1=szm[:, jv, :],
                                                                    op=ALU.mult), [b_t2, b_szm], [b_y])
                    stop_at(4)
                    def pool_gen(gq):
                        wlog = gq + 1
                        pv, bp = inproj2(3080 + gq * 256)
                        xpg = xp[:, 2 * gq:2 * gq + 2, :]
                        cx.op("act", lambda e: e.copy(out=xpg[:, :, 16:16 + T], in_=pv), [bp], [b_xp[gq]])
                        yield
                        pv, bp = inproj2(4104 + gq * 256)
                        cx.op("act", lambda e: e.activation(out=szp[:], in_=pv, func=AF.Silu), [bp], [b_szp])
                        yield
                        cur, bcur = xpg, b_xp[gq]
                        tgl = [(pA, b_pA), (pB, b_pB)]
                        for st_ in range(wlog):
                            sh = 1 << st_
                            lo = 2 * sh
                            dst, bdst = tgl[st_ % 2]
                            cx.op("pool", lambda e: e.tensor_tensor(out=dst[:, :, lo:16 + T], in0=cur[:, :, lo:16 + T],
                                                                    in1=cur[:, :, lo - sh:16 + T - sh], op=ALU.add),
                                  [bcur], [bdst])
                            cur, bcur = dst, bdst
                        if blk == 0:
                            cx.op("pool", lambda e: e.tensor_tensor(
                                out=cur[:, :, 16:32], in0=cur[:, :, 16:32],
                                in1=pfix[:, gq * 16:(gq + 1) * 16].unsqueeze(1).to_broadcast([128, 2, 16]), op=ALU.mult),
                                [bcur, b_pfix], [bcur])
                        cx.op("dve", lambda e: e.scalar_tensor_tensor(out=mixb[:], in0=cur[:, :, 16:16 + T],
                                                                      scalar=1.0 / (1 << wlog), in1=xpg[:, :, 16:16 + T],
                                                                      op0=ALU.mult, op1=ALU.subtract),
                              [bcur, b_xp[gq]], [b_mix])
                        cx.op("pool", lambda e: e.tensor_copy(out=xpg[:, :, 0:16], in_=xpg[:, :, T:T + 16]),
                              [b_xp[gq]], [b_xp[gq]])
                        yield
                        g, bg = nextG()
                        for oc in range(2):
                            cx.mm(g[:, oc * T:(oc + 1) * T],
                                  [(wp_v[:, gq, cc, oc * 128:(oc + 1) * 128], mixb[:, cc, :]) for cc in range(2)],
                                  cur_w + [b_mix], [bg])
                        for oc in range(2):
                            ch = 2 * gq + oc
                            cx.op("dve", lambda e: e.scalar_tensor_tensor(out=y[:, 8 + ch, :], in0=g[:, oc * T:(oc + 1) * T],
                                                                          scalar=vec[:, V_PS + ch:V_PS + ch + 1],
                                                                          in1=szp[:, oc, :], op0=ALU.mult, op1=ALU.mult),
                                  [bg, b_vec, b_szp], [b_y])
                    for h_ in range(4):
                        interleave(head_gen(h_), pool_gen(h_))
                    stop_at(5)
                    outproj_store(wo_v, 16, s, blk, par)
                    stop_at(6)
            cx.barrier()
            lst.close()

        def layer_c(j, src):
            w = W["c", j]
            lst = ExitStack()
            open_lst.append(lst)

            def sb(name, shape, dt=F32):
                return lst.enter_context(nc.sbuf_tensor(name + "_c%d" % j, list(shape), dt))

            cvec = sb("cvec", [128, 4]); b_cvec = cx.buf("cvec")
            posi = sb("posi", [64, T], I32); b_posi = cx.buf("posi")
            ua = sb("ua", [64, T]); b_ua = cx.buf("ua")
            ub = sb("ub", [64, T]); b_ub = cx.buf("ub")
            ki = sb("ki", [64, T], I32); b_ki = cx.buf("ki")
            tA = sb("tA", [64, T]); b_tA = cx.buf("tA")
            tB = sb("tB", [64, T]); b_tB = cx.buf("tB")
            sinT = sb("sinT", [64, T]); b_sin = cx.buf("sin")
            cosT = sb("cosT", [64, T]); b_cos = cx.buf("cos")
            cq32 = sb("cq32", [128, 3, T]); b_cq32 = cx.buf("cq32")
            cqn = sb("cqn", [128, 3, T], BF16); b_cqn = cx.buf("cqn")
            ckvn = sb("ckvn", [128, 2, T], BF16); b_ckvn = cx.buf("ckvn")
            sqc = sb("sqc", [128, 3, T], BF16); b_sqc = cx.buf("sqc")
            rsc = sb("rsc", [128, T]); b_rsc = cx.buf("rsc")
            sqkp = sb("sqkp", [64, T], BF16); b_sqkp = cx.buf("sqkp")
            sqk = sb("sqk", [128, 2, T], BF16); b_sqk = cx.buf("sqk")
            kss = sb("kss", [128, 16]); b_kss = cx.buf("kss")
            kscale = sb("kscale", [128, 16, 8]); b_ksc = cx.buf("kscale")
            sqq = sb("sqq", [128, T], BF16); b_sqq = cx.buf("sqq")
            sqqr = sb("sqqr", [64, T], BF16); b_sqqr = cx.buf("sqqr")
            rq = sb("rq", [128, T]); b_rq = cx.buf("rq")
            qn = [sb("qn%d" % i, [128, T], BF16) for i in range(2)]; b_qn = [cx.buf("qn%d" % i) for i in range(2)]
            qr = [sb("qr%d" % i, [64, T], BF16) for i in range(2)]; b_qr = [cx.buf("qr%d" % i) for i in range(2)]
            qtmp = sb("qtmp", [128, T]); b_qtmp = cx.buf("qtmp")
            szall = sb("szall", [128, 8, T], BF16); b_sz = cx.buf("sz")
            Pt = [sb("Pt%d" % i, [128, T], BF16) for i in range(2)]; b_Pt = [cx.buf("Pt%d" % i) for i in range(2)]
            rinv = sb("rinv", [128, T]); b_rinv = cx.buf("rinv")
            ob = sb("ob", [128, T]); b_ob = cx.buf("ob")
            O_ps, b_O = P4[0], b_P4[0]
            R_ps, b_R = P4[1], b_P4[1]
            K_ps, b_K = P4[2], b_P4[2]
            Sb = [(P4[2], b_P4[2]), (P4[3], b_P4[3])]
            x32.append(sb("x32b", [128, 8, T])); b_x32.append(cx.buf("x32b"))
            cx.barrier()
            del cur_w[:]
            cx.dma(vec[:, 0:19], w["vec"][:, :], [], [b_vec], b_vec)
            cx.op("dve", lambda e: e.tensor_tensor(out=cvec[:, 0:1], in0=vec[:, 13:14], in1=vec[:, 14:15], op=ALU.mult),
                  [b_vec], [b_cvec])
            cx.op("dve", lambda e: e.tensor_tensor(out=cvec[0:64, 1:2], in0=vec[0:64, 16:17], in1=freq[:, 1:2], op=ALU.mult),
                  [b_vec, b_freq], [b_cvec])
            cx.op("dve", lambda e: e.tensor_tensor(out=cvec[0:64, 2:3], in0=vec[0:64, 18:19], in1=freq[:, 1:2], op=ALU.mult),
                  [b_vec, b_freq], [b_cvec])
            o = 0
            win_v = arena[:, o:o + 8 * 1792].rearrange("p (k n) -> p k n", k=8); o += 8 * 1792
            wuq_v = arena[:, o:o + 3 * 2048].rearrange("p (k n) -> p k n", k=3); o += 3 * 2048
            wukv_v = arena[:, o:o + 2 * 2048].rearrange("p (k n) -> p k n", k=2); o += 2 * 2048
            wo_v = arena[:, o:o + 8 * D].rearrange("p (k n) -> p k n", k=8); o += 8 * D
            KT = arena[:, o:o + 8 * S].rearrange("p (h t) -> p h t", h=8); o += 8 * S
            V = arena[:, o:o + 16 * 1024].rearrange("p (j n) -> p j n", j=16); o += 16 * 1024
            kr = arena[:, o:o + S]; o += S
            assert o <= WCOLS
            b_KT = cx.buf("KT"); b_V = cx.buf("V"); b_kr = cx.buf("kr")
            win_src = w["win"].rearrange("(k p) n -> p k n", p=128)
            for kc in range(8):
                load_w(win_v[:, kc, :], win_src[:, kc, :], 1792, vec[:, kc:kc + 1], b_vec)
            wuq_src = w["wuq"].rearrange("(k p) n -> p k n", p=128)
            for kc in range(3):
                load_w(wuq_v[:, kc, :], wuq_src[:, kc, :], 2048, vec[:, 8 + kc:9 + kc], b_vec)
            wukv_src = w["wukv"].rearrange("(k p) n -> p k n", p=128)
            for kc in range(2):
                load_w(wukv_v[:, kc, :], wukv_src[:, kc, :], 2048, vec[:, 11 + kc:12 + kc], b_vec)
            wo_src = w["wout"].rearrange("(k p) n -> p k n", p=128)
            for kc in range(8):
                load_w(wo_v[:, kc, :], wo_src[:, kc, :], D)

            def inp(dst_ps, col0, m, bg):
                cx.mm(dst_ps, [(win_v[:, kc, col0:col0 + m], xn[:, kc, :]) for kc in range(8)], cur_w + [b_xn], [bg])

            order = [(s_, b_) for s_ in range(ns) for b_ in range(NB)]
            load_block(src, 0, 0, 0)
            for it_, (s, blk) in enumerate(order):
                if True:
                    par = it_ % 2
                    t0 = blk * T
                    if it_ + 1 < len(order):
                        load_block(src, order[it_ + 1][0], order[it_ + 1][1], 1 - par)
                    norm_block(par)
                    stop_at(21)
                    pos_ap = bass.AP(pos.tensor, s * S + t0, [[0, 64], [1, T]])
                    cx.dma(posi[:], pos_ap, [], [b_posi], b_posi)
                    cx.op("dve", lambda e: e.tensor_copy(out=ua[:], in_=posi[:]), [b_posi], [b_ua])
                    cx.op("dve", lambda e: e.tensor_scalar(out=ua[:], in0=ua[:], scalar1=freq[:, 0:1], scalar2=None,
                                                           op0=ALU.mult), [b_ua, b_freq], [b_ua])
                    cx.op("dve", lambda e: e.tensor_scalar(out=ub[:], in0=ua[:], scalar1=0.25, scalar2=None, op0=ALU.add),
                          [b_ua], [b_ub])
                    for (su, bsu, dst, bdst) in ((ua, b_ua, sinT, b_sin), (ub, b_ub, cosT, b_cos)):
                        cx.op("dve", lambda e: e.tensor_copy(out=ki[:], in_=su[:]), [bsu], [b_ki])
                        cx.op("dve", lambda e: e.tensor_copy(out=tA[:], in_=ki[:]), [b_ki], [b_tA])
                        cx.op("dve", lambda e: e.tensor_tensor(out=tB[:], in0=su[:], in1=tA[:], op=ALU.subtract),
                              [bsu, b_tA], [b_tB])
                        cx.op("act", lambda e: e.activation(out=dst[:], in_=tB[:], func=AF.Sin, scale=2.0 * PI),
                              [b_tB], [bdst])
                    stop_at(22)
                    for hp in range(4):
                        g, bg = nextG()
                        for i in range(2):
                            inp(g[:, i * T:(i + 1) * T], 704 + (2 * hp + i) * 128, 128, bg)
                        cx.op("act", lambda e: e.activation(out=szall[:, 2 * hp:2 * hp + 2, :],
                                                            in_=g[:, 0:2 * T].rearrange("p (a t) -> p a t", a=2),
                                                            func=AF.Silu), [bg], [b_sz])
                    stop_at(23)
                    g, bg = nextG()
                    inp(g[:, 0:T], 0, 128, bg)
                    inp(g[:, T:2 * T], 128, 128, bg)
                    g2, bg2 = nextG()
                    inp(g2[:, 0:T], 256, 128, bg2)
                    cx.op("act", lambda e: e.copy(out=cq32[:, 0:2, :], in_=g[:, 0:2 * T].rearrange("p (a t) -> p a t", a=2)),
                          [bg], [b_cq32])
                    cx.op("act", lambda e: e.copy(out=cq32[:, 2, :], in_=g2[:, 0:T]), [bg2], [b_cq32])
                    cx.op("act", lambda e: e.activation(out=sqc[:], in_=cq32[:], func=AF.Square), [b_cq32], [b_sqc])
                    g3, bg3 = nextG()
                    cx.mm(g3[:, 0:T], [(onesb[:], sqc[:, kc, :]) for kc in range(3)], [b_onesb, b_sqc], [bg3])
                    rstd_from_ps(g3[:, 0:T], bg3, rsc[:], b_rsc, 1.0 / 384, EPS)
                    for kc in range(3):
                        cx.op("dve", lambda e: e.tensor_tensor(out=cqn[:, kc, :], in0=cq32[:, kc, :],
                                                                                  in1=rsc[:], op=ALU.mult),
                              [b_cq32, b_rsc], [b_cqn])
                    g, bg = nextG()
                    inp(g[:, 0:T], 384, 128, bg)
                    inp(g[:, T:2 * T], 512, 128, bg)
                    cx.op("act", lambda e: e.copy(out=cq32[:, 0:2, :], in_=g[:, 0:2 * T].rearrange("p (a t) -> p a t", a=2)),
                          [bg], [b_cq32])
                    cx.op("act", lambda e: e.activation(out=sqc[:, 0:2, :], in_=cq32[:, 0:2, :], func=AF.Square),
                          [b_cq32], [b_sqc])
                    g3, bg3 = nextG()
                    cx.mm(g3[:, 0:T], [(onesb[:], sqc[:, kc, :]) for kc in range(2)], [b_onesb, b_sqc], [bg3])
                    rstd_from_ps(g3[:, 0:T], bg3, rsc[:], b_rsc, 1.0 / 256, EPS)
                    for kc in range(2):
                        cx.op("dve", lambda e: e.tensor_tensor(out=ckvn[:, kc, :], in0=cq32[:, kc, :],
                                                                                  in1=rsc[:], op=ALU.mult),
                              [b_cq32, b_rsc], [b_ckvn])
                    stop_at(24)
                    g, bg = nextG()
                    inp(g[0:64, 0:T], 640, 64, bg)
                    inp(g[0:64, T:2 * T], 1728, 64, bg)
                    stop_at(31)
                    cx.op("act", lambda e: e.copy(out=ua[:], in_=g[0:64, 0:T]), [bg], [b_ua])
                    cx.op("act", lambda e: e.copy(out=ub[:], in_=g[0:64, T:2 * T]), [bg], [b_ub])
                    cx.op("dve", lambda e: e.scalar_tensor_tensor(out=tA[:], in0=ua[:], scalar=vec[0:64, 17:18],
                                                                  in1=cosT[:], op0=ALU.mult, op1=ALU.mult),
                          [b_ua, b_vec, b_cos], [b_tA])
                    stop_at(32)
                    cx.op("dve", lambda e: e.scalar_tensor_tensor(out=tB[:], in0=ub[:], scalar=cvec[0:64, 2:3],
                                                                  in1=sinT[:], op0=ALU.mult, op1=ALU.mult),
                          [b_ub, b_cvec, b_sin], [b_tB])
                    stop_at(33)
                    cx.op("dve", lambda e: e.tensor_tensor(out=kr[0:64, t0:t0 + T], in0=tA[:], in1=tB[:], op=ALU.add),
                          [b_tA, b_tB], [b_kr])
                    stop_at(34)
                    cx.op("act", lambda e: e.activation(out=sqkp[:], in_=ua[:], func=AF.Square), [b_ua], [b_sqkp])
                    stop_at(25)
                    for hp in range(4):
                        g, bg = nextG()
                        for i in range(2):
                            h = 2 * hp + i
                            cx.mm(g[:, i * T:(i + 1) * T],
                                  [(wukv_v[:, kc, h * 128:(h + 1) * 128], ckvn[:, kc, :]) for kc in range(2)],
                                  cur_w + [b_ckvn], [bg])
                        gv = g[:, 0:2 * T].rearrange("p (a t) -> p a t", a=2)
                        cx.op("act", lambda e: e.copy(out=KT[:, 2 * hp:2 * hp + 2, t0:t0 + T], in_=gv), [bg], [b_KT])
                        cx.op("act", lambda e: e.activation(out=sqk[:], in_=gv, func=AF.Square), [bg], [b_sqk])
                        for st2 in range(2):
                            for i in range(2):
                                h = 2 * hp + i
                                cx.mm(K_ps[:, st2 * 8 + h:st2 * 8 + h + 1],
                                      [(sqk[:, i, st2 * 128:(st2 + 1) * 128], onesb[:, 0:1]),
                                       (sqkp[0:64, st2 * 128:(st2 + 1) * 128], onesb[0:64, 0:1])],
                                      [b_sqk, b_sqkp, b_onesb], [b_K])
                    cx.op("dve", lambda e: e.tensor_scalar(out=kss[:], in0=K_ps[:, 0:16], scalar1=1.0 / 192, scalar2=EPS,
                                                           op0=ALU.mult, op1=ALU.add), [b_K], [b_kss])
                    cx.op("act", lambda e: e.activation(out=kss[:], in_=kss[:], func=AF.Ln), [b_kss], [b_kss])
                    cx.op("act", lambda e: e.activation(out=kss[:], in_=kss[:], func=AF.Exp, scale=-0.5), [b_kss], [b_kss])
                    cx.op("dve", lambda e: e.tensor_scalar(out=kscale[:, 2 * blk:2 * blk + 2, :],
                                                           in0=kss[:].rearrange("p (a h) -> p a h", a=2),
                                                           scalar1=192.0 ** -0.5, scalar2=None, op0=ALU.mult),
                          [b_kss], [b_ksc])
                    stop_at(26)
                    for st2 in range(2):
                        for half in range(2):
                            g, bg = nextG()
                            cx.mm(g[:, 0:512],
                                  [(ckvn[:, kc, st2 * 128:(st2 + 1) * 128],
                                    wukv_v[:, kc, 1024 + half * 512:1024 + (half + 1) * 512]) for kc in range(2)],
                                  cur_w + [b_ckvn], [bg])
                            if half == 0:
                                cx.op("act", lambda e: e.copy(out=V[:, 2 * blk + st2, 0:512], in_=g[:, 0:512]), [bg], [b_V])
                            else:
                                cx.op("dve", lambda e: e.tensor_copy(out=V[:, 2 * blk + st2, 512:1024], in_=g[:, 0:512]),
                                      [bg], [b_V])
                    stop_at(27)
                    nkt = 2 * blk + 2

                    def qprep(h, qb):
                        g, bg = nextG()
                        cx.mm(g[:, 0:T], [(wuq_v[:, kc, h * 128:(h + 1) * 128], cqn[:, kc, :]) for kc in range(3)],
                              cur_w + [b_cqn], [bg])
                        g2, bg2 = nextG()
                        cx.mm(g2[0:64, 0:T], [(wuq_v[:, kc, 1024 + h * 64:1024 + (h + 1) * 64], cqn[:, kc, :]) for kc in range(3)],
                              cur_w + [b_cqn], [bg2])
                        cx.mm(g2[0:64, T:2 * T], [(wuq_v[:, kc, 1536 + h * 64:1536 + (h + 1) * 64], cqn[:, kc, :]) for kc in range(3)],
                              cur_w + [b_cqn], [bg2])
                        cx.op("act", lambda e: e.copy(out=qtmp[:], in_=g[:, 0:T]), [bg], [b_qtmp])
                        cx.op("act", lambda e: e.copy(out=ua[:], in_=g2[0:64, 0:T]), [bg2], [b_ua])
                        cx.op("act", lambda e: e.copy(out=ub[:], in_=g2[0:64, T:2 * T]), [bg2], [b_ub])
                        yield
                        cx.op("act", lambda e: e.activation(out=sqq[:], in_=qtmp[:], func=AF.Square), [b_qtmp], [b_sqq])
                        cx.op("act", lambda e: e.activation(out=sqqr[:], in_=ua[:], func=AF.Square), [b_ua], [b_sqqr])
                        yield
                        g3, bg3 = nextG()
                        cx.mm(g3[:, 0:T], [(onesb[:], sqq[:]), (onesb[0:64, :], sqqr[:])], [b_onesb, b_sqq, b_sqqr], [bg3])
                        rstd_from_ps(g3[:, 0:T], bg3, rq[:], b_rq, 1.0 / 192, EPS)
                        yield
                        cx.op("dve", lambda e: e.scalar_tensor_tensor(out=qn[qb][:], in0=qtmp[:], scalar=cvec[:, 0:1], in1=rq[:],
                                                                      op0=ALU.mult, op1=ALU.mult),
                              [b_qtmp, b_cvec, b_rq], [b_qn[qb]])
                        cx.op("dve", lambda e: e.scalar_tensor_tensor(out=tA[:], in0=ua[:], scalar=vec[0:64, 15:16],
                                                                      in1=cosT[:], op0=ALU.mult, op1=ALU.mult),
                              [b_ua, b_vec, b_cos], [b_tA])
                        yield
                        cx.op("dve", lambda e: e.scalar_tensor_tensor(out=tB[:], in0=ub[:], scalar=cvec[0:64, 1:2],
                                                                      in1=sinT[:], op0=ALU.mult, op1=ALU.mult),
                              [b_ub, b_cvec, b_sin], [b_tB])
                        cx.op("dve", lambda e: e.tensor_tensor(out=tA[:], in0=tA[:], in1=tB[:], op=ALU.add),
                              [b_tA, b_tB], [b_tA])
                        cx.op("dve", lambda e: e.tensor_tensor(out=qr[qb][:], in0=tA[:], in1=rq[0:64, :], op=ALU.mult),
                              [b_tA, b_rq], [b_qr[qb]])
                        yield

                    def attend(h, qb):
                        def s_mm(jt_):
                            gS_, bS_ = Sb[jt_ % 2]
                            cx.mm(gS_[:, 0:T], [(KT[:, h, jt_ * 128:(jt_ + 1) * 128], qn[qb][:]),
                                                (kr[0:64, jt_ * 128:(jt_ + 1) * 128], qr[qb][:])],
                                  [b_KT, b_kr, b_qn[qb], b_qr[qb]], [bS_])
                            return gS_, bS_
                        pend = s_mm(0)
                        for jt in range(nkt):
                            gS, bS = pend
                            if jt + 1 < nkt:
                                pend = s_mm(jt + 1)
                            pi = jt % 2
                            cx.op("act", lambda e: e.activation(out=Pt[pi][:], in_=gS[:, 0:T], func=AF.Exp,
                                                                scale=kscale[:, jt, h:h + 1]), [bS, b_ksc], [b_Pt[pi]])
                            if jt >= 2 * blk:
                                d_ = jt - 2 * blk
                                cx.op("dve", lambda e: e.tensor_tensor(out=Pt[pi][:], in0=Pt[pi][:],
                                                                       in1=amask[:, d_ * T:(d_ + 1) * T], op=ALU.mult),
                                      [b_Pt[pi], b_am], [b_Pt[pi]])
                            cx.mm(O_ps[:, 0:T], [(V[:, jt, h * 128:(h + 1) * 128], Pt[pi][:])], [b_V, b_Pt[pi]], [b_O],
                                  start=(jt == 0), stop=(jt == nkt - 1))
                            cx.mm(R_ps[:, 0:T], [(onesb[:], Pt[pi][:])], [b_onesb, b_Pt[pi]], [b_R],
                                  start=(jt == 0), stop=(jt == nkt - 1))
                            yield
                        cx.op("dve", lambda e: e.reciprocal(out=rinv[:], in_=R_ps[:, 0:T]), [b_R], [b_rinv])
                        cx.op("dve", lambda e: e.tensor_tensor(out=ob[:], in0=O_ps[:, 0:T], in1=rinv[:], op=ALU.mult),
                              [b_O, b_rinv], [b_ob])
                        cx.op("dve", lambda e: e.tensor_tensor(out=y[:, h, :], in0=ob[:], in1=szall[:, h, :], op=ALU.mult),
                              [b_ob, b_sz], [b_y])
                        yield

                    interleave(qprep(0, 0))
                    for h_ in range(8):
                        interleave(attend(h_, h_ % 2), qprep(h_ + 1, (h_ + 1) % 2) if h_ + 1 < 8 else None)
                    stop_at(29)
                    outproj_store(wo_v, 8, s, blk, par)
            cx.barrier()
            x32.pop(); b_x32.pop()
            lst.close()

        try:
            for li, (kind, j) in enumerate(layers):
                src = xT if li == 0 else oT
                if kind == "a":
                    layer_a(j, src)
                else:
                    layer_c(j, src)
        except StopBuild:
            for l_ in open_lst:
                l_.close()
        cx.final_wait()
    return nc


def _consts():
    c = {}
    c["c_ones"] = np.ones((128, 128), np.float32)
    s_ = np.arange(64)
    c["c_tri"] = (s_[:, None] <= s_[None, :]).astype(np.float32)
    id4 = np.zeros((4, 8), np.float32)
    id4[:, 0:4] = np.eye(4)
    id4[:, 4:8] = np.eye(4) / 16.0
    c["c_id4"] = id4
    sel = np.zeros((4, 4, 128), np.float32)
    for h in range(4):
        sel[h, h, :] = 1.0
    c["c_sel"] = sel.reshape(4, 512)
    jj = np.arange(64)
    inv = (10000.0 ** (-(np.arange(0, 64, 2, dtype=np.float32)) / 64.0)).astype(np.float32)
    fr = np.zeros((64, 2), np.float32)
    fr[:, 0] = (inv.astype(np.float64)[jj % 32] / (2.0 * np.pi)).astype(np.float32)
    fr[:, 1] = np.where(jj < 32, -1.0, 1.0)
    c["c_freq"] = fr
    am = np.zeros((128, 2, T), np.float32)
    s2 = np.arange(128)[:, None] // 64
    tq = np.arange(T)[None, :] // 64
    for d_ in range(2):
        am[:, d_, :] = ((2 * d_ + s2) <= tq).astype(np.float32)
    c["c_amask"] = am.reshape(128, 2 * T)
    pf = np.ones((128, 64), np.float32)
    for g, w in enumerate((2, 4, 8, 16)):
        t = np.arange(16)
        pf[:, g * 16:(g + 1) * 16] = (w / np.minimum(t + 1, w)).astype(np.float32)[None, :]
    c["c_pfix"] = pf
    return c


def _pvec(v):
    v = np.asarray(v, np.float32)
    return np.ascontiguousarray(v.reshape(-1, 128).T)


def _prep_a(inp, j):
    d = {}
    d["a_win%d" % j] = np.ascontiguousarray(inp["a_w_in"][j], dtype=np.float32)
    d["a_wq%d" % j] = np.ascontiguousarray(inp["a_w_q"][j], dtype=np.float32)
    d["a_wk%d" % j] = np.ascontiguousarray(inp["a_w_k"][j], dtype=np.float32)
    d["a_wv%d" % j] = np.ascontiguousarray(inp["a_w_v"][j], dtype=np.float32)
    d["a_wpool%d" % j] = np.ascontiguousarray(inp["a_pool_w"][j], dtype=np.float32)
    d["a_wout%d" % j] = np.ascontiguousarray(inp["a_w_out"][j], dtype=np.float32)
    cw = inp["a_conv_w"][j]
    vec = np.concatenate([_pvec(inp["a_norm_g"][j])] + [_pvec(cw[k]) for k in range(4)] +
                         [_pvec(inp["a_conv_b"][j]), _pvec(inp["a_head_norm_g"][j]), _pvec(inp["a_skip"][j]),
                          _pvec(inp["a_pool_scale"][j])], axis=1)
    d["a_vec%d" % j] = np.ascontiguousarray(vec, dtype=np.float32)
    d["a_bif%d" % j] = np.ascontiguousarray(np.asarray(inp["a_b_if"][j], np.float32).reshape(2, 4).T)
    return d


def _prep_c(inp, j):
    d = {}
    perm = (np.arange(64) + 32) % 64
    win = np.asarray(inp["c_w_in"][j], np.float32)
    d["c_win%d" % j] = np.ascontiguousarray(np.concatenate([win, win[:, 640 + perm]], axis=1))
    wuq = np.asarray(inp["c_w_uq"][j], np.float32)
    hh_ = np.arange(8)[:, None]
    nope = (hh_ * 192 + np.arange(128)[None, :]).reshape(-1)
    rope = (hh_ * 192 + 128 + np.arange(64)[None, :]).reshape(-1)
    rot = (hh_ * 192 + 128 + perm[None, :]).reshape(-1)
    d["c_wuq%d" % j] = np.ascontiguousarray(wuq[:, np.concatenate([nope, rope, rot])])
    wukv = np.asarray(inp["c_w_ukv"][j], np.float32)
    kidx = (hh_ * 256 + np.arange(128)[None, :]).reshape(-1)
    vidx = (hh_ * 256 + 128 + np.arange(128)[None, :]).reshape(-1)
    d["c_wukv%d" % j] = np.ascontiguousarray(wukv[:, np.concatenate([kidx, vidx])])
    d["c_wout%d" % j] = np.ascontiguousarray(inp["c_w_out"][j], dtype=np.float32)
    qg = np.asarray(inp["c_qn_g"][j], np.float32)
    kg = np.asarray(inp["c_kn_g"][j], np.float32)

    def col64(v):
        o_ = np.zeros((128, 1), np.float32)
        o_[:64, 0] = v
        return o_
    vec = np.concatenate([_pvec(inp["c_norm_g"][j]), _pvec(inp["c_q_norm_g"][j]), _pvec(inp["c_kv_norm_g"][j]),
                          qg[:128, None], kg[:128, None], col64(qg[128:]), col64(qg[128:][perm]),
                          col64(kg[128:]), col64(kg[128:][perm])], axis=1)
    d["c_vec%d" % j] = np.ascontiguousarray(vec, dtype=np.float32)
    return d


LAYERS = [("a", 0), ("c", 0), ("a", 1), ("c", 1)]
_CACHE = {}


def run(inputs, layers, ns, n_cores):
    key = (tuple(layers), ns)
    if key not in _CACHE:
        _CACHE[key] = build_program(ns, layers)
    nc = _CACHE[key]
    x = np.asarray(inputs["x"], np.float32)
    posi = np.asarray(inputs["positions"], np.int32)
    shared = dict(_consts())
    for j in sorted({j for k, j in layers if k == "a"}):
        shared.update(_prep_a(inputs, j))
    for j in sorted({j for k, j in layers if k == "c"}):
        shared.update(_prep_c(inputs, j))
    in_maps = []
    for c in range(n_cores):
        m = dict(shared)
        m["xT"] = np.ascontiguousarray(x[c * ns:(c + 1) * ns].transpose(0, 2, 1))
        m["pos"] = np.ascontiguousarray(posi[c * ns:(c + 1) * ns])
        in_maps.append(m)
    res = run_bass_kernel_spmd(nc, in_maps, core_ids=list(range(n_cores)))
    outs = [np.asarray(r["oT"]).transpose(0, 2, 1) for r in res.results]
    return np.ascontiguousarray(np.concatenate(outs, axis=0), dtype=np.float32)


def kernel(**inputs):
    return run(inputs, LAYERS, 4, 8)
```
